# Optimizing a Trainium2 kernel written in Bass

```python
import math
import jax, jax.numpy as jnp
from jax import lax
import numpy as np

D_MODEL = 2048
BATCH = 16
SEQ = 2048
DEPTH = 1

PLE_DIM = 256
MIX_WIDTH = D_MODEL
LRU_WIDTH = MIX_WIDTH // 2
LRU_BLOCKS = 16
LRU_BLOCK_DIM = LRU_WIDTH // LRU_BLOCKS
CONV_WIDTH = 4
LRU_C = 8.0
HEAD_DIM = 64
N_HEADS = (MIX_WIDTH - LRU_WIDTH) // HEAD_DIM
N_KV = 4
HEADS_PER_KV = N_HEADS // N_KV
KV_W = N_KV * HEAD_DIM
CMP_LEN = 32
CMP_STRIDE = 16
CMP_HIDDEN = 256
SEL_BLOCK = 64
N_SELECT = 16
N_LOCAL_FORCED = 2
WINDOW = 512
Q_BLOCK = 128
SEL_Q_CHUNK = 32
N_BUCKETS = 32
MAX_DISTANCE = 128
D_FF = 4 * D_MODEL
NORM_EPS = 1e-6
NEG_INF = -1e30
FORCE_SCORE = 1e4

IN_SIZES = (LRU_WIDTH, LRU_WIDTH, N_HEADS * HEAD_DIM, KV_W, KV_W, KV_W, KV_W, KV_W, KV_W, 3 * N_HEADS)
IN_DIM = sum(IN_SIZES)
SPLIT_POINTS = tuple(int(v) for v in np.cumsum(IN_SIZES)[:-1])

kernel_name = "hymba_rglru_nsa_hybrid_layer"


def rms_norm(x, g):
    xf = x.astype(jnp.float32)
    y = xf * lax.rsqrt(jnp.mean(xf * xf, axis=-1, keepdims=True) + NORM_EPS)
    return (y * g.astype(jnp.float32)).astype(x.dtype)


def rel_bucket(dist):
    n = jnp.maximum(dist, 0)
    max_exact = N_BUCKETS // 2
    nf = jnp.maximum(n, 1).astype(jnp.float32)
    large = max_exact + (jnp.log(nf / max_exact) / math.log(MAX_DISTANCE / max_exact)
                         * (N_BUCKETS - max_exact)).astype(jnp.int32)
    large = jnp.minimum(large, N_BUCKETS - 1)
    return jnp.where(n < max_exact, n, large)


def rglru_mixer(u, gate_in, conv_w, conv_b, wa, ba, wx, bx, lam):
    B, S = u.shape[0], u.shape[1]
    up = jnp.pad(u, ((0, 0), (CONV_WIDTH - 1, 0), (0, 0)))
    xc = conv_b + sum(up[:, k:k + S] * conv_w[k] for k in range(CONV_WIDTH))
    xb = xc.reshape(B, S, LRU_BLOCKS, LRU_BLOCK_DIM)
    r = jax.nn.sigmoid(jnp.einsum('bsni,nij->bsnj', xb, wa) + ba).reshape(B, S, LRU_WIDTH)
    ig = jax.nn.sigmoid(jnp.einsum('bsni,nij->bsnj', xb, wx) + bx).reshape(B, S, LRU_WIDTH)
    log_a = -LRU_C * r.astype(jnp.float32) * jax.nn.softplus(-lam.astype(jnp.float32))
    a = jnp.exp(log_a)
    b = jnp.sqrt(-jnp.expm1(2.0 * log_a)) * (ig * xc).astype(jnp.float32)

    def combine(left, right):
        a1, b1 = left
        a2, b2 = right
        return a1 * a2, a2 * b1 + b2

    _, h = lax.associative_scan(combine, (a, b), axis=1)
    return h.astype(u.dtype) * jax.nn.gelu(gate_in)


def nsa_mixer(q, k_c, v_c, k_s, v_s, k_w, v_w, gates,
              pe_k, w1_k, w2_k, pe_v, w1_v, w2_v, rel_bias):
    B, S = q.shape[0], q.shape[1]
    G, R, dk = N_KV, HEADS_PER_KV, HEAD_DIM
    q = q.reshape(B, S, G, R, dk)
    k_c, v_c, k_s, v_s, k_w, v_w = [t.reshape(B, S, G, dk) for t in (k_c, v_c, k_s, v_s, k_w, v_w)]
    scale = dk ** -0.5
    pos = jnp.arange(S, dtype=jnp.int32)
    bias_tab = rel_bias.astype(jnp.float32).reshape(N_BUCKETS, G, R)

    n_cmp = (S - CMP_LEN) // CMP_STRIDE + 1
    cmp_idx = np.arange(n_cmp)[:, None] * CMP_STRIDE + np.arange(CMP_LEN)[None, :]

    def compress(t, pe, w1, w2):
        blk = t[:, cmp_idx] + pe[:, None, :]
        blk = blk.transpose(0, 1, 3, 2, 4).reshape(B, n_cmp, G, CMP_LEN * dk)
        return jax.nn.gelu(blk @ w1) @ w2

    kc = compress(k_c, pe_k, w1_k, w2_k)
    vc = compress(v_c, pe_v, w1_v, w2_v)
    cmp_end = jnp.asarray(np.arange(n_cmp) * CMP_STRIDE + CMP_LEN - 1, dtype=jnp.int32)
    dist_c = pos[:, None] - cmp_end[None, :]
    valid_c = dist_c >= 0
    bias_c = bias_tab[rel_bucket(dist_c)].transpose(2, 3, 0, 1)
    s_c = jnp.einsum('bsgrd,bngd->bgrsn', q, kc).astype(jnp.float32) * scale + bias_c
    s_c = jnp.where(valid_c, s_c, NEG_INF)
    any_c = (pos >= CMP_LEN - 1)[:, None]
    p_c = jax.nn.softmax(s_c, axis=-1) * any_c
    o_c = jnp.einsum('bgrsn,bngd->bsgrd', p_c.astype(vc.dtype), vc)

    n_sel = S // SEL_BLOCK
    ratio_sel = SEL_BLOCK // CMP_STRIDE
    ratio_cmp = CMP_LEN // CMP_STRIDE
    jj_np = np.arange(n_sel)[:, None, None]
    ci = ratio_sel * jj_np + np.arange(ratio_sel)[None, :, None] - np.arange(ratio_cmp)[None, None, :]
    jb = np.broadcast_to(jj_np, ci.shape)
    ok = (ci >= 0) & (ci < n_cmp)
    M = np.zeros((n_cmp, n_sel), np.float32)
    np.add.at(M, (ci[ok], jb[ok]), 1.0)
    imp = jnp.einsum('bgrsn,nj->bgsj', p_c, jnp.asarray(M))
    jj = jnp.arange(n_sel, dtype=jnp.int32)
    dblk = (pos // SEL_BLOCK)[:, None] - jj[None, :]
    forced = (jj[None, :] == 0) | ((dblk >= 0) & (dblk < N_LOCAL_FORCED))
    causal_blk = dblk >= 0
    imp = jnp.where(forced, FORCE_SCORE, jnp.where(causal_blk, imp, -FORCE_SCORE))
    n_top = min(N_SELECT, n_sel)
    _, sel_idx = lax.top_k(imp, n_top)

    kb = k_s.reshape(B, n_sel, SEL_BLOCK, G, dk).transpose(0, 3, 1, 2, 4)
    vb = v_s.reshape(B, n_sel, SEL_BLOCK, G, dk).transpose(0, 3, 1, 2, 4)
    n_ch = S // SEL_Q_CHUNK
    q_ch = q.reshape(B, n_ch, SEL_Q_CHUNK, G, R, dk).transpose(1, 0, 2, 3, 4, 5)
    idx_ch = sel_idx.reshape(B, G, n_ch, SEL_Q_CHUNK, n_top).transpose(2, 0, 1, 3, 4)
    pos_ch = pos.reshape(n_ch, SEL_Q_CHUNK)
    b_ar = jnp.arange(B)[:, None, None]
    g_ar = jnp.arange(G)[None, :, None]
    g_ar4 = jnp.arange(G)[None, :, None, None]
    n_keys = n_top * SEL_BLOCK
    in_blk = jnp.arange(SEL_BLOCK, dtype=jnp.int32)

    def sel_chunk(args):
        qc, ic, tc = args
        flat = ic.reshape(B, G, SEL_Q_CHUNK * n_top)
        ks = kb[b_ar, g_ar, flat].reshape(B, G, SEL_Q_CHUNK, n_keys, dk)
        vs = vb[b_ar, g_ar, flat].reshape(B, G, SEL_Q_CHUNK, n_keys, dk)
        kpos = (ic[..., None] * SEL_BLOCK + in_blk).reshape(B, G, SEL_Q_CHUNK, n_keys)
        dist = tc[None, None, :, None] - kpos
        bias = bias_tab[rel_bucket(dist), g_ar4].transpose(0, 1, 4, 2, 3)
        s = jnp.einsum('bqgrd,bgqkd->bgrqk', qc, ks).astype(jnp.float32) * scale + bias
        s = jnp.where((dist >= 0)[:, :, None], s, NEG_INF)
        pr = jax.nn.softmax(s, axis=-1)
        return jnp.einsum('bgrqk,bgqkd->bqgrd', pr.astype(vs.dtype), vs)

    o_s = lax.map(sel_chunk, (q_ch, idx_ch, pos_ch))
    o_s = o_s.transpose(1, 0, 2, 3, 4, 5).reshape(B, S, G, R, dk)

    n_qb = S // Q_BLOCK
    band = Q_BLOCK + WINDOW
    kw_pad = jnp.pad(k_w, ((0, 0), (WINDOW, 0), (0, 0), (0, 0)))
    vw_pad = jnp.pad(v_w, ((0, 0), (WINDOW, 0), (0, 0), (0, 0)))
    kj = jnp.arange(band, dtype=jnp.int32)[None, :]
    dist_w = WINDOW + jnp.arange(Q_BLOCK, dtype=jnp.int32)[:, None] - kj
    band_ok = (dist_w >= 0) & (dist_w < WINDOW)
    bias_w = bias_tab[rel_bucket(dist_w)].transpose(2, 3, 0, 1)
    q_blk = q.reshape(B, n_qb, Q_BLOCK, G, R, dk).transpose(1, 0, 2, 3, 4, 5)
    starts = jnp.arange(n_qb, dtype=jnp.int32) * Q_BLOCK

    def win_block(args):
        qb, st = args
        kw = lax.dynamic_slice_in_dim(kw_pad, st, band, axis=1)
        vw = lax.dynamic_slice_in_dim(vw_pad, st, band, axis=1)
        ok_w = band_ok & (st - WINDOW + kj >= 0)
        s = jnp.einsum('bqgrd,bkgd->bgrqk', qb, kw).astype(jnp.float32) * scale + bias_w
        s = jnp.where(ok_w, s, NEG_INF)
        pr = jax.nn.softmax(s, axis=-1)
        return jnp.einsum('bgrqk,bkgd->bqgrd', pr.astype(vw.dtype), vw)

    o_w = lax.map(win_block, (q_blk, starts))
    o_w = o_w.transpose(1, 0, 2, 3, 4, 5).reshape(B, S, G, R, dk)

    gt = jax.nn.sigmoid(gates).reshape(B, S, G, R, 3)
    o = gt[..., 0:1] * o_c + gt[..., 1:2] * o_s + gt[..., 2:3] * o_w
    return o.reshape(B, S, N_HEADS * dk)


def setup_inputs(seed: int = 0) -> dict:
    key = jax.random.key(seed)
    ks = jax.random.split(key, 32)
    f32 = jnp.float32

    def nrm(k, shape, scale):
        return jax.random.normal(k, shape, f32) * scale

    def gain(k, shape):
        return 1.0 + 0.05 * jax.random.normal(k, shape, f32)

    a0 = jax.random.uniform(ks[10], (DEPTH, LRU_WIDTH), f32, 0.9, 0.999)
    return {
        "x": nrm(ks[0], (BATCH, SEQ, D_MODEL), 1.0),
        "p": nrm(ks[1], (DEPTH, BATCH, SEQ, PLE_DIM), 1.0),
        "norm_mix_pre": gain(ks[2], (DEPTH, D_MODEL)),
        "norm_mix_post": gain(ks[3], (DEPTH, D_MODEL)),
        "norm_mlp_pre": gain(ks[4], (DEPTH, D_MODEL)),
        "norm_mlp_post": gain(ks[5], (DEPTH, D_MODEL)),
        "w_in": nrm(ks[6], (DEPTH, D_MODEL, IN_DIM), D_MODEL ** -0.5),
        "conv_w": nrm(ks[7], (DEPTH, CONV_WIDTH, LRU_WIDTH), CONV_WIDTH ** -0.5),
        "conv_b": nrm(ks[8], (DEPTH, LRU_WIDTH), 0.02),
        "lru_wa": nrm(ks[9], (DEPTH, LRU_BLOCKS, LRU_BLOCK_DIM, LRU_BLOCK_DIM), LRU_BLOCK_DIM ** -0.5),
        "lru_ba": nrm(ks[11], (DEPTH, LRU_BLOCKS, LRU_BLOCK_DIM), 0.02),
        "lru_wx": nrm(ks[12], (DEPTH, LRU_BLOCKS, LRU_BLOCK_DIM, LRU_BLOCK_DIM), LRU_BLOCK_DIM ** -0.5),
        "lru_bx": nrm(ks[13], (DEPTH, LRU_BLOCKS, LRU_BLOCK_DIM), 0.02),
        "lru_lambda": jnp.log(a0 / (1.0 - a0)),
        "cmp_pe_k": nrm(ks[14], (DEPTH, CMP_LEN, HEAD_DIM), 0.1),
        "cmp_w1_k": nrm(ks[15], (DEPTH, CMP_LEN * HEAD_DIM, CMP_HIDDEN), (CMP_LEN * HEAD_DIM) ** -0.5),
        "cmp_w2_k": nrm(ks[16], (DEPTH, CMP_HIDDEN, HEAD_DIM), CMP_HIDDEN ** -0.5),
        "cmp_pe_v": nrm(ks[17], (DEPTH, CMP_LEN, HEAD_DIM), 0.1),
        "cmp_w1_v": nrm(ks[18], (DEPTH, CMP_LEN * HEAD_DIM, CMP_HIDDEN), (CMP_LEN * HEAD_DIM) ** -0.5),
        "cmp_w2_v": nrm(ks[19], (DEPTH, CMP_HIDDEN, HEAD_DIM), CMP_HIDDEN ** -0.5),
        "rel_bias": nrm(ks[20], (N_BUCKETS, N_HEADS), 0.1),
        "gnorm_lru": gain(ks[21], (DEPTH, LRU_WIDTH)),
        "gnorm_nsa": gain(ks[22], (DEPTH, N_HEADS * HEAD_DIM)),
        "w_out": nrm(ks[23], (DEPTH, MIX_WIDTH, D_MODEL), MIX_WIDTH ** -0.5),
        "mlp_w1": nrm(ks[24], (DEPTH, D_MODEL, D_FF), D_MODEL ** -0.5),
        "mlp_w2": nrm(ks[25], (DEPTH, D_FF, D_MODEL), D_FF ** -0.5),
        "ple_gate": nrm(ks[26], (DEPTH, D_MODEL, D_MODEL), D_MODEL ** -0.5),
        "ple_proj": nrm(ks[27], (DEPTH, PLE_DIM, D_MODEL), PLE_DIM ** -0.5),
    }


def reference(x, p, norm_mix_pre, norm_mix_post, norm_mlp_pre, norm_mlp_post, w_in,
              conv_w, conv_b, lru_wa, lru_ba, lru_wx, lru_bx, lru_lambda,
              cmp_pe_k, cmp_w1_k, cmp_w2_k, cmp_pe_v, cmp_w1_v, cmp_w2_v, rel_bias,
              gnorm_lru, gnorm_nsa, w_out, mlp_w1, mlp_w2, ple_gate, ple_proj):
    h = x
    for i in range(DEPTH):
        a = rms_norm(h, norm_mix_pre[i])
        z = a @ w_in[i]
        u, g_lru, q, kc, vc, ksel, vsel, kw, vw, gts = jnp.split(z, SPLIT_POINTS, axis=-1)
        y_lru = rglru_mixer(u, g_lru, conv_w[i], conv_b[i], lru_wa[i], lru_ba[i],
                            lru_wx[i], lru_bx[i], lru_lambda[i])
        y_nsa = nsa_mixer(q, kc, vc, ksel, vsel, kw, vw, gts,
                          cmp_pe_k[i], cmp_w1_k[i], cmp_w2_k[i],
                          cmp_pe_v[i], cmp_w1_v[i], cmp_w2_v[i], rel_bias)
        y = jnp.concatenate([rms_norm(y_lru, gnorm_lru[i]), rms_norm(y_nsa, gnorm_nsa[i])], axis=-1)
        h = h + rms_norm(y @ w_out[i], norm_mix_post[i])
        f = jnp.square(jax.nn.relu(rms_norm(h, norm_mlp_pre[i]) @ mlp_w1[i])) @ mlp_w2[i]
        h = h + rms_norm(f, norm_mlp_post[i])
        h = h + jax.nn.sigmoid(h @ ple_gate[i]) * (p[i] @ ple_proj[i])
    return h
```

```python
from contextlib import ExitStack
import math
import numpy as np
import ml_dtypes
import concourse.bass as bass
import concourse.mybir as mybir
from concourse.bass_utils import run_bass_kernel_spmd

F32 = mybir.dt.float32
BF16 = mybir.dt.bfloat16
AF = mybir.ActivationFunctionType
ALU = mybir.AluOpType

ENGS = ("pe", "act", "dve", "pool", "sp")
T = 2048
D = 2048
NTT = 16
EPS = 1e-6
GC1 = 0.044715
GC2 = 1.5957691216057308


class Sched:
    def __init__(self, nc):
        self.nc = nc
        self.streams = {e: [] for e in ENGS}
        self.count = {e: 0 for e in ENGS}
        self.last_w = {}
        self.readers = {}
        self.waited = {e: {} for e in ENGS}
        self.nd = {"sp": 14, "pool": 8}
        self.dma_rr = {e: 0 for e in self.nd}
        self.dma_cnt = {e: [0] * self.nd[e] for e in self.nd}
        self.sems = {}
        self.n_instr = 0

    @staticmethod
    def _ev_sem(ev):
        if ev[0] == "c":
            return ("c", ev[1]), ev[2]
        return ("d", ev[1], ev[2]), ev[3]

    def add(self, eng, fn, reads=(), writes=(), dma=False):
        deps = set()
        for k in reads:
            for w_ in self.last_w.get(k, ()):
                deps.add(w_)
            if isinstance(k, str) and k.startswith("pb") and k[2:].isdigit():
                for r in self.readers.get(k, ()):
                    if not (r[0] == "c" and r[1] == eng):
                        deps.add(r)
        for k in writes:
            for w_ in self.last_w.get(k, ()):
                if dma and w_[0] == "d":
                    continue
                deps.add(w_)
            for r in self.readers.get(k, ()):
                deps.add(r)
        need = {}
        for ev in deps:
            if ev[0] == "c" and ev[1] == eng and eng == "pe":
                continue
            s, v = self._ev_sem(ev)
            if need.get(s, 0) < v:
                need[s] = v
        if dma:
            idx = self.dma_rr[eng]
            self.dma_rr[eng] = (idx + 1) % self.nd[eng]
            prev = self.dma_cnt[eng][idx]
            self.dma_cnt[eng][idx] = prev + 16
            if prev > 0:
                s = ("d", eng, idx)
                if need.get(s, 0) < prev:
                    need[s] = prev
            myev = ("d", eng, idx, prev + 16)
            inc = (("d", eng, idx), 16)
        else:
            self.count[eng] += 1
            myev = ("c", eng, self.count[eng])
            inc = (("c", eng), 1)
        waits = []
        wd = self.waited[eng]
        for s, v in need.items():
            if wd.get(s, 0) >= v:
                continue
            wd[s] = v
            waits.append((s, v))
        self.streams[eng].append((waits, fn, inc))
        self.n_instr += 1
        for k in reads:
            self.readers.setdefault(k, []).append(myev)
        for k in writes:
            prevw = self.last_w.get(k, [])
            if dma and prevw and prevw[0][0] == "d" and not self.readers.get(k):
                self.last_w[k] = prevw + [myev]
            else:
                self.last_w[k] = [myev]
            self.readers[k] = []
        return myev

    def _all_now(self):
        cur = []
        for e in ENGS:
            if self.count[e] > 0:
                cur.append((("c", e), self.count[e]))
        for e in self.nd:
            for i in range(self.nd[e]):
                if self.dma_cnt[e][i] > 0:
                    cur.append((("d", e, i), self.dma_cnt[e][i]))
        return cur

    def barrier(self, engines=ENGS):
        cur = self._all_now()
        for e in engines:
            wd = self.waited[e]
            waits = []
            for s, v in cur:
                if wd.get(s, 0) >= v:
                    continue
                wd[s] = v
                waits.append((s, v))
            if waits:
                self.streams[e].append((waits, None, None))
        self.last_w = {}
        self.readers = {}

    def emit(self, stack):
        nc = self.nc
        semkeys = [("c", e) for e in ENGS]
        for e in self.nd:
            for i in range(self.nd[e]):
                semkeys.append(("d", e, i))
        for k in semkeys:
            self.sems[k] = stack.enter_context(nc.semaphore("s_" + "_".join(str(x) for x in k)))
        block = stack.enter_context(nc.Block())
        sems = self.sems

        def run(engobj, stream):
            for waits, fn, inc in stream:
                for s, v in waits:
                    engobj.wait_ge(sems[s], v)
                if fn is None:
                    continue
                ins = fn(engobj)
                ins.then_inc(sems[inc[0]], inc[1])

        @block.tensor
        def _(e):
            run(e, self.streams["pe"])

        @block.scalar
        def _(e):
            run(e, self.streams["act"])

        @block.vector
        def _(e):
            run(e, self.streams["dve"])

        @block.gpsimd
        def _(e):
            run(e, self.streams["pool"])

        @block.sync
        def _(e):
            run(e, self.streams["sp"])


class Arena:
    def __init__(self, t, words):
        self.t = t
        self.words = words
        self.off = 0

    def f32(self, n):
        assert self.off + n <= self.words, ("arena overflow", self.off, n)
        v = self.t[:, self.off:self.off + n]
        self.off += n
        return v

    def bf(self, n):
        w = (n + 1) // 2
        assert self.off + w <= self.words, ("arena overflow", self.off, w)
        v = self.t[:, self.off:self.off + w].bitcast(BF16)
        self.off += w
        return v

    def mark(self):
        return self.off

    def reset(self, m):
        self.off = m


V_GPRE, V_GMLP, V_GLRU, V_GNSA, V_CW, V_CB, V_BA, V_BX, V_LAM, V_C31 = 0, 16, 32, 40, 48, 80, 88, 96, 104, 112
NVEC = 128

IN_OFF = dict(u=0, g=1024, q=2048, kc=3072, vc=3328, ks=3584, vs=3840, kw=4096, vw=4352, gt=4608)


def build(nseq=2, dbg=False, stop_after=None):
    nc = bass.Bass("TRN2", target_bir_lowering=False)

    def din(name, shape, dt=F32):
        return nc.dram_tensor(name, list(shape), dt, kind="ExternalInput").ap()

    x = din("x", [nseq, T, D])
    pin = din("p", [nseq, T, 256])
    w_in_g = din("w_in_g", [4, D, 652])
    w_in_l = din("w_in_l", [8, D, 256])
    w_out = din("w_out", [D, D])
    w1 = din("mlp_w1", [D, 8192])
    w2 = din("mlp_w2", [8192, D])
    wg = din("ple_gate", [D, D])
    wp = din("ple_proj", [256, D])
    cw1k = din("cmp_w1_k", [2048, 256])
    cw1v = din("cmp_w1_v", [2048, 256])
    cw2k = din("cmp_w2_k", [256, 64])
    cw2v = din("cmp_w2_v", [256, 64])
    pekT = din("pekT", [64, 32])
    pevT = din("pevT", [64, 32])
    vecs = din("vecs", [128, NVEC])
    grow = din("grow", [128, 2, D])
    bd = din("bd", [128, 2, 8, 128])
    ident_d = din("ident", [128, 128], BF16)
    tbn = din("tb_near", [128, 2, 16, 128])
    maskw_d = din("maskw", [128, 128], BF16)
    tbc = din("tb_c", [4, 128, 16, 4, 128])
    cmfm_d = din("cmfm", [128, 2, 16, 32])
    ex_d = din("ex", [32, T], BF16)
    M_d = din("Mmat", [127, 32], BF16)
    out = nc.dram_tensor("out", [nseq, T, D], F32, kind="ExternalOutput").ap()

    ws_out = nc.dram_tensor("ws_out", [D, D], BF16).ap()
    ws_1 = nc.dram_tensor("ws_1", [D, 8192], BF16).ap()
    ws_2 = nc.dram_tensor("ws_2", [8192, D], BF16).ap()
    ws_g = nc.dram_tensor("ws_g", [D, D], BF16).ap()
    ws_p = nc.dram_tensor("ws_p", [256, D], BF16).ap()
    ws_c1 = nc.dram_tensor("ws_c1", [2, 64, 32, 256], BF16).ap()
    if dbg:
        yT_s = nc.dram_tensor("yT_s", [nseq, 128, 16, T], BF16, kind="ExternalOutput").ap()
        onsa_s = nc.dram_tensor("onsa_s", [nseq, T, 1024], F32, kind="ExternalOutput").ap()
    else:
        yT_s = nc.dram_tensor("yT_s", [nseq, 128, 16, T], BF16).ap()
        onsa_s = nc.dram_tensor("onsa_s", [nseq, T, 1024], F32).ap()

    S = Sched(nc)

    with ExitStack() as st:
        AW = 49200
        arena_t = st.enter_context(nc.sbuf_tensor("arena", [128, AW], F32))
        cst_t = st.enter_context(nc.sbuf_tensor("cst", [128, 3968], F32))
        AR = Arena(arena_t, AW)
        CS = Arena(cst_t, 3968)
        pbank = [st.enter_context(nc.psum_tensor(f"pb{i}", [128, 512], F32)) for i in range(8)]

        def PB(i):
            return pbank[i][:]

        def PBb(i):
            return pbank[i][:].bitcast(BF16)

        def pk(i):
            return f"pb{i}"

        def mm(o, lhsT, rhs, start, stop, r, w, **kw):
            S.add("pe", lambda e: e.matmul(o, lhsT=lhsT, rhs=rhs, start=start, stop=stop, **kw), r, w)

        def tr(o, in_, r, w):
            S.add("pe", lambda e: e.transpose(out=o, in_=in_, identity=ident[:, :]), list(r) + ["ident"], w)

        def act(o, in_, func, r, w, **kw):
            S.add("act", lambda e: e.activation(out=o, in_=in_, func=func, **kw), r, w)

        def tt(eng, o, a, b, op, r, w):
            S.add(eng, lambda e: e.tensor_tensor(out=o, in0=a, in1=b, op=op), r, w)

        def ts(eng, o, a, s1, s2, op0, op1, r, w, **kw):
            if s2 is None:
                S.add(eng, lambda e: e.tensor_scalar(out=o, in0=a, scalar1=s1, scalar2=None, op0=op0, **kw), r, w)
            else:
                S.add(eng, lambda e: e.tensor_scalar(out=o, in0=a, scalar1=s1, scalar2=s2, op0=op0, op1=op1, **kw), r, w)

        def stt(o, a, sc, b, op0, op1, r, w, **kw):
            S.add("dve", lambda e: e.scalar_tensor_tensor(out=o, in0=a, scalar=sc, in1=b, op0=op0, op1=op1, **kw), r, w)

        def cp(eng, o, a, r, w):
            S.add(eng, lambda e: e.tensor_copy(out=o, in_=a), r, w)

        def memset(eng, o, val, w):
            S.add(eng, lambda e: e.memset(o, val), (), w)

        def recip(o, a, r, w):
            S.add("dve", lambda e: e.reciprocal(out=o, in_=a), r, w)

        def dma(q, o, in_, r, w):
            return S.add(q, lambda e: e.dma_start(out=o, in_=in_), r, w, dma=True)

        ident = CS.bf(128)
        maskw = CS.bf(128)
        vec = CS.f32(NVEC)
        negc = CS.f32(16)
        cl = CS.f32(8)
        cltmp = CS.f32(8)
        enear = CS.bf(2 * 16 * 128).rearrange("p (d h q) -> p d h q", d=2, h=16)
        cmfm = CS.f32(2 * 16 * 32).rearrange("p (a t j) -> p a t j", a=2, t=16)
        ones_f = CS.f32(128)
        zer_b = CS.bf(264)
        maskw4 = CS.bf(512).rearrange("p (r q) -> p r q", r=4)
        neg8c = CS.f32(16)
        nbab = CS.f32(16)
        stats = CS.f32(32)
        dma("sp", ident, ident_d, (), ["ident"])
        dma("sp", maskw, maskw_d, (), ["maskw"])
        dma("sp", vec, vecs, (), ["vec"])
        dma("sp", cmfm, cmfm_d, (), ["cmfm"])
        memset("dve", ones_f, 1.0, ["ones_f"])
        memset("dve", zer_b, 0.0, ["zer_b"])
        ts("dve", negc, vec[:, V_C31:V_C31 + 16], -1.0, None, ALU.mult, None, ["vec"], ["negc"])
        ts("dve", neg8c, vec[:, V_C31:V_C31 + 16], -8.0, None, ALU.mult, None, ["vec"], ["neg8c"])
        ts("dve", nbab, vec[:, V_BA:V_BA + 16], -1.0, None, ALU.mult, None, ["vec"], ["nbab"])
        cp("dve", maskw4, maskw.unsqueeze(1).to_broadcast([128, 4, 128]), ["maskw"], ["maskw4"])
        act(cltmp, vec[:, V_LAM:V_LAM + 8], AF.Exp, ["vec"], ["cltmp"], scale=-1.0)
        act(cltmp, cltmp, AF.Ln, ["cltmp"], ["cltmp"], bias=1.0, scale=1.0)
        ts("dve", cl, cltmp, -8.0, None, ALU.mult, None, ["cltmp"], ["cl"])
        m0 = AR.mark()
        stg = AR.f32(2 * 16 * 128).rearrange("p (d h q) -> p d h q", d=2, h=16)
        dma("sp", stg, tbn, (), ["stg"])
        for h in range(16):
            act(enear[:, :, h, :], stg[:, :, h, :], AF.Identity, ["stg", "neg8c"], ["enear"], bias=neg8c[:, h:h + 1], scale=8.0)
        S.barrier()
        AR.reset(m0)

        dma("pool", ws_c1[0], cw1k.rearrange("(l d) h -> d l h", d=64), (), ["ws_c1"])
        dma("pool", ws_c1[1], cw1v.rearrange("(l d) h -> d l h", d=64), (), ["ws_c1"])
        precast_list = []

        def precast(dst, src, rows, step, key):
            for r0 in range(0, rows, step):
                precast_list.append(lambda r0=r0: dma("pool", dst[r0:r0 + step, :], src[r0:r0 + step, :], (), [key]))

        precast(ws_out, w_out, D, 512, "ws_out")
        precast(ws_g, wg, D, 512, "ws_g")
        precast(ws_p, wp, 256, 256, "ws_p")
        precast(ws_1, w1, D, 128, "ws_1")
        precast(ws_2, w2, 8192, 512, "ws_2")
        precast_it = iter(precast_list)

        for sq in range(nseq):
            S.barrier()
            AR.reset(0)
            aT = AR.bf(16 * T).rearrange("p (k t) -> p k t", k=16)
            base_m = AR.mark()

            xt = [AR.f32(D) for _ in range(2)]
            junk = AR.bf(D)
            abf = [AR.bf(D) for _ in range(2)]

            def rms_to_T(src_tile, src_key, dstT, dst_key, tok0, gcol, par, st_off, pbs=(0, 1), nk=16, junk_ap=None, junk_key="junk", scale_on_act=False):
                ssq = stats[:, st_off:st_off + 1]
                std = stats[:, st_off + 1:st_off + 2]
                rstd = stats[:, st_off + 2:st_off + 3]
                kk = f"st{st_off}"
                jk = junk[:, 0:nk * 128] if junk_ap is None else junk_ap
                act(jk, src_tile, AF.Square, [src_key], [junk_key, kk + "a"], accum_out=ssq)
                act(std, ssq, AF.Ln, [kk + "a"], [kk + "b"], bias=EPS, scale=1.0 / (nk * 128))
                act(rstd, std, AF.Exp, [kk + "b"], [kk + "c"], scale=-0.5)
                ab = abf[par]
                if scale_on_act:
                    act(ab[:, 0:nk * 128], src_tile, AF.Copy, [src_key, kk + "c"], [f"abf{par}"], scale=rstd)
                else:
                    ts("dve", ab[:, 0:nk * 128], src_tile, rstd, None, ALU.mult, None, [src_key, kk + "c"], [f"abf{par}"])
                for k4 in range(nk // 4):
                    b = pbs[k4 % 2]
                    pt = PBb(b)[:, 0:512].rearrange("p (j q) -> p j q", j=4)
                    for j in range(4):
                        kc = k4 * 4 + j
                        tr(pt[:, j, :], ab[:, kc * 128:(kc + 1) * 128], [f"abf{par}"], [pk(b)])
                    if gcol is None:
                        cp("dve", dstT[:, k4 * 4:(k4 + 1) * 4, tok0:tok0 + 128], pt, [pk(b)], [dst_key])
                    else:
                        gv = vec[:, gcol + k4 * 4:gcol + (k4 + 1) * 4].unsqueeze(2).to_broadcast([128, 4, 128])
                        tt("dve", dstT[:, k4 * 4:(k4 + 1) * 4, tok0:tok0 + 128], pt, gv, ALU.mult, [pk(b), "vec"], [dst_key])

            for t in range(NTT):
                par = t % 2
                dma("sp", xt[par], x[sq, t * 128:(t + 1) * 128, :], (), [f"xt{par}"])
                rms_to_T(xt[par], f"xt{par}", aT, "aT", t * 128, V_GPRE, par, 3 * par)
            S.barrier()
            AR.reset(base_m)

            ylru = AR.bf(8 * T).rearrange("p (c t) -> p c t", c=8)
            bdt = AR.f32(2 * 8 * 128).rearrange("p (a c m) -> p a c m", a=2, c=8)
            dma("sp", bdt, bd, (), ["bdt"])
            wl = [AR.bf(16 * 2 * 128).rearrange("p (k a c) -> p k a c", k=16, a=2) for _ in range(2)]
            upad = [AR.f32(3 + T) for _ in range(2)]
            NTMP = 9
            m_tmp = AR.mark()
            tmp = [[AR.f32(512) for _ in range(NTMP)] for _ in range(2)]
            hbuf = [AR.f32(512) for _ in range(2)]
            ysq_ring = [AR.bf(512) for _ in range(4)]
            ones_b = AR.bf(128)
            memset("pool", ones_b, 1.0, ["ones_b"])

            def lru_wload(c):
                dma("pool", wl[c % 2], w_in_l[c].rearrange("(k p) (a c) -> p k a c", p=128, a=2), (), [f"wl{c%2}"])

            def lru_s1(c, tp):
                wb, wk, up, uk = wl[c % 2], f"wl{c%2}", upad[c % 2], f"upad{c%2}"
                pr = (c * 4 + tp) % 2
                X = tmp[pr]
                tk = lambda i: f"tmp{pr}_{i}"
                tok = slice(tp * 512, (tp + 1) * 512)
                if tp == 0:
                    if c + 1 < 8:
                        lru_wload(c + 1)
                    memset("pool", up[:, 0:3], 0.0, [uk])
                for kc in range(16):
                    mm(PB(0), wb[:, kc, 0, :], aT[:, kc, tok], kc == 0, kc == 15, [wk, "aT"], [pk(0)])
                cp("dve", up[:, 3 + tp * 512: 3 + (tp + 1) * 512], PB(0), [pk(0)], [uk])
                for kc in range(16):
                    mm(PB(1), wb[:, kc, 1, :], aT[:, kc, tok], kc == 0, kc == 15, [wk, "aT"], [pk(1)])
                gg, t1 = X[7], X[8]
                act(gg, PB(1), AF.Copy, [pk(1)], [tk(7)])
                act(t1, PB(1), AF.Square, [pk(1)], [tk(8)])
                xc = X[0]
                cwc = lambda k: vec[:, V_CW + c * 4 + k: V_CW + c * 4 + k + 1]
                ts("dve", xc, up[:, tp * 512: tp * 512 + 512], cwc(0), vec[:, V_CB + c:V_CB + c + 1], ALU.mult, ALU.add, [uk, "vec"], [tk(0)])
                for k in range(1, 4):
                    stt(xc, up[:, tp * 512 + k: tp * 512 + k + 512], cwc(k), xc, ALU.mult, ALU.add, [uk, "vec", tk(0)], [tk(0)])
                ts("dve", t1, t1, GC1, 1.0, ALU.mult, ALU.add, [tk(8)], [tk(8)])
                tt("pool", t1, t1, gg, ALU.mult, [tk(8), tk(7)], [tk(8)])

            def lru_s2(c, tp):
                pr = (c * 4 + tp) % 2
                X = tmp[pr]
                tk = lambda i: f"tmp{pr}_{i}"
                tok = slice(tp * 512, (tp + 1) * 512)
                xc, gg, t1 = X[0], X[7], X[8]
                mm(PB(2), bdt[:, 0, c, :], xc, True, True, ["bdt", tk(0)], [pk(2)])
                mm(PB(3), bdt[:, 1, c, :], xc, True, True, ["bdt", tk(0)], [pk(3)])
                rr, ig, aa, ss, bb = X[1], X[2], X[3], X[4], X[5]
                act(rr, PB(2), AF.Sigmoid, [pk(2), "vec"], [tk(1)], bias=vec[:, V_BA + c:V_BA + c + 1], scale=1.0)
                act(ig, PB(3), AF.Sigmoid, [pk(3), "vec"], [tk(2)], bias=vec[:, V_BX + c:V_BX + c + 1], scale=1.0)
                act(t1, t1, AF.Sigmoid, [tk(8)], [tk(8)], scale=GC2)
                act(aa, rr, AF.Exp, [tk(1), "cl"], [tk(3)], scale=cl[:, c:c + 1])
                tt("pool", ss, aa, aa, ALU.mult, [tk(3)], [tk(4)])
                act(ss, ss, AF.Ln, [tk(4)], [tk(4)], bias=1.0, scale=-1.0)
                act(ss, ss, AF.Exp, [tk(4)], [tk(4)], scale=0.5)
                tt("pool", bb, ig, xc, ALU.mult, [tk(2), tk(0)], [tk(5)])
                tt("pool", bb, bb, ss, ALU.mult, [tk(5), tk(4)], [tk(5)])
                hh = hbuf[pr]
                hprev = hbuf[1 - pr]
                if tp == 0:
                    S.add("dve", lambda e, hh=hh, aa=aa, bb=bb: e.tensor_tensor_scan(out=hh, data0=aa, data1=bb, initial=0.0, op0=ALU.mult, op1=ALU.add),
                          [tk(3), tk(5)], [f"h{pr}"])
                else:
                    S.add("dve", lambda e, hh=hh, aa=aa, bb=bb, hprev=hprev: e.tensor_tensor_scan(out=hh, data0=aa, data1=bb, initial=hprev[:, 511:512], op0=ALU.mult, op1=ALU.add),
                          [tk(3), tk(5), f"h{1-pr}"], [f"h{pr}"])
                tt("pool", gg, gg, t1, ALU.mult, [tk(7), tk(8)], [tk(7)])
                yy = X[6]
                ri = (c * 4 + tp) % 4
                tt("dve", yy, hh, gg, ALU.mult, [f"h{pr}", tk(7)], [tk(6)])
                act(ysq_ring[ri], yy, AF.Square, [tk(6)], [f"ysq{ri}"])
                act(ylru[:, c, tok], yy, AF.Copy, [tk(6)], ["ylru"])

            def lru_s3(c, tp):
                ri = (c * 4 + tp) % 4
                mm(PB(4 + tp), ones_b, ysq_ring[ri], c == 0, c == 7, ["ones_b", f"ysq{ri}"], [pk(4 + tp)])

            lru_wload(0)
            pieces = [(c, tp) for c in range(8) for tp in range(4)]
            for i in range(len(pieces) + 3):
                if i < len(pieces):
                    lru_s1(*pieces[i])
                if 0 <= i - 1 < len(pieces):
                    lru_s2(*pieces[i - 1])
                if 0 <= i - 3 < len(pieces):
                    lru_s3(*pieces[i - 3])
            S.barrier()
            AR.reset(m_tmp)
            rl = AR.f32(T)
            ynb = [AR.bf(T) for _ in range(2)]
            for tp in range(4):
                act(rl[:, tp * 512:(tp + 1) * 512], PB(4 + tp), AF.Ln, [pk(4 + tp)], ["rl"], bias=EPS, scale=1.0 / 1024)
            act(rl, rl, AF.Exp, ["rl"], ["rl"], scale=-0.5)
            for c in range(8):
                yb = ynb[c % 2]
                stt(yb, ylru[:, c, :], vec[:, V_GLRU + c:V_GLRU + c + 1], rl, ALU.mult, ALU.mult, ["ylru", "vec", "rl"], [f"ynb{c%2}"])
                dma("sp", yT_s[sq, :, c, :], yb, [f"ynb{c%2}"], ["yT_s"])
            S.barrier()
            AR.reset(base_m)

            qT = AR.bf(4 * T).rearrange("p (r t) -> p r t", r=4)
            ksT = AR.bf(T)
            kwT = AR.bf(T)
            kcT = AR.bf(T)
            vcT = AR.bf(T)
            vsA = AR.bf(NTT * 65).rearrange("p (t d) -> p t d", t=NTT)
            vwA = AR.bf(NTT * 65).rearrange("p (t d) -> p t d", t=NTT)
            gsig = AR.f32(NTT * 12).rearrange("p (t c) -> p t c", t=NTT)
            kcmpT = AR.bf(128)
            vcA = AR.bf(98)
            hidT = AR.bf(2 * 128).rearrange("p (k n) -> p k n", k=2)
            w1c = AR.bf(32 * 256).rearrange("p (l h) -> p l h", l=32)
            w2c = AR.bf(2 * 64).rearrange("p (k d) -> p k d", k=2)
            peT = AR.bf(32)
            pbias = AR.f32(2)
            gtmp = [AR.f32(128) for _ in range(3)]
            Ec = AR.bf(16 * 4 * 128).rearrange("p (t r i) -> p t r i", t=16, r=4)
            ecs = AR.f32(4 * 4 * 128).rearrange("p (t r i) -> p t r i", t=4, r=4)
            wgrp = AR.bf(16 * 652).rearrange("p (k c) -> p k c", k=16)
            wq = wgrp[:, :, 0:256]
            wtok = wgrp[:, :, 512:652]
            NPT = 5
            Pt = [AR.bf(512) for _ in range(NPT)]
            etmp = [AR.f32(512) for _ in range(3)]
            otile = [AR.f32(256).rearrange("p (r d) -> p r d", r=4) for _ in range(2)]
            imp = AR.f32(32)
            impw = AR.f32(32)
            m8 = AR.f32(16)
            negm = [AR.bf(32) for _ in range(2)]
            sml = AR.f32(32)
            ot2b = {8: AR.f32(256), 12: AR.f32(256)}
            memset("dve", vsA[:, :, 64:65], 1.0, ["vsA"])
            memset("dve", vwA[:, :, 64:65], 1.0, ["vwA"])
            memset("dve", vcA[:, 64:65], 1.0, ["vcA"])
            dma("sp", vcA[0:127, 65:97], M_d, (), ["vcA"])
            dma("sp", ksT[64:96, :], ex_d, (), ["ksT"])

            def nsa_wload(g):
                dma("pool", wgrp, w_in_g[g].rearrange("(k p) c -> p k c", p=128), (), ["wgrp"])

            for g in range(4):
                if g == 0:
                    nsa_wload(0)
                for qq in range(4):
                    dma("sp", ecs, tbc[g, :, qq * 4:(qq + 1) * 4, :, :], (), ["ecs"])
                    for r in range(4):
                        act(Ec[:, qq * 4:(qq + 1) * 4, r, :], ecs[:, :, r, :], AF.Identity, ["ecs", "neg8c"], ["Ec"],
                            bias=neg8c[:, 4 * g + r:4 * g + r + 1], scale=8.0)
                fm_pairs = [(wgrp[:, :, 0:128], qT[0:64, 0, :], "qT", qT[0:64, 1, :], "qT"),
                            (wgrp[:, :, 128:256], qT[0:64, 2, :], "qT", qT[0:64, 3, :], "qT"),
                            (wgrp[:, :, 256:384], kcT[0:64, :], "kcT", vcT[0:64, :], "vcT"),
                            (wgrp[:, :, 384:512], ksT[0:64, :], "ksT", kwT[0:64, :], "kwT")]
                n_ = 0
                for (wsrc, dA, kA, dB, kB) in fm_pairs:
                    for tp in range(4):
                        b = n_ % 2
                        n_ += 1
                        tok = slice(tp * 512, (tp + 1) * 512)
                        for kc in range(16):
                            mm(PB(b), wsrc[:, kc, :], aT[:, kc, tok], kc == 0, kc == 15, ["wgrp", "aT"], [pk(b)])
                        act(dA[:, tok], PB(b)[0:64, :], AF.Copy, [pk(b)], [kA])
                        cp("dve", dB[:, tok], PB(b)[64:128, :], [pk(b)], [kB])
                for t in range(NTT):
                    b = 2 + t % 2
                    for kc in range(16):
                        mm(PB(b)[:, 0:140], aT[:, kc, t * 128:(t + 1) * 128], wtok[:, kc, :], kc == 0, kc == 15, ["aT", "wgrp"], [pk(b)])
                    cp("dve", vsA[:, t, 0:64], PB(b)[:, 0:64], [pk(b)], ["vsA"])
                    cp("dve", vwA[:, t, 0:64], PB(b)[:, 64:128], [pk(b)], ["vwA"])
                    act(gsig[:, t, :], PB(b)[:, 128:140], AF.Sigmoid, [pk(b)], ["gsig"])
                if g + 1 < 4:
                    nsa_wload(g + 1)
                for which, (cw1, cw2, pe_d, srcT) in enumerate(((cw1k, cw2k, pekT, kcT), (cw1v, cw2v, pevT, vcT))):
                    skey = "kcT" if which == 0 else "vcT"
                    dma("sp", w1c[0:64], ws_c1[which], ["ws_c1"], ["w1c"])
                    dma("pool", w2c, cw2.rearrange("(k p) d -> p k d", p=128), (), ["w2c"])
                    dma("pool", peT[0:64, :], pe_d, (), ["peT"])
                    for hk in range(2):
                        for l in range(32):
                            mm(PB(4)[:, 0:1], w1c[0:64, l, hk * 128:(hk + 1) * 128], peT[0:64, l:l + 1], l == 0, l == 31, ["w1c", "peT"], [pk(4)])
                        cp("dve", pbias[:, hk:hk + 1], PB(4)[:, 0:1], [pk(4)], ["pbias"])
                        for l in range(32):
                            mm(PB(5)[:, 0:127], w1c[0:64, l, hk * 128:(hk + 1) * 128], srcT[0:64, l:l + 16 * 126 + 1:16], l == 0, l == 31, ["w1c", skey], [pk(5)])
                        xg, t1, t2 = gtmp[0][:, 0:127], gtmp[1][:, 0:127], gtmp[2][:, 0:127]
                        act(xg, PB(5)[:, 0:127], AF.Identity, [pk(5), "pbias"], ["gt0"], bias=pbias[:, hk:hk + 1], scale=1.0)
                        act(t1, xg, AF.Square, ["gt0"], ["gt1"])
                        ts("dve", t1, t1, GC1, 1.0, ALU.mult, ALU.add, ["gt1"], ["gt1"])
                        tt("dve", t1, t1, xg, ALU.mult, ["gt1", "gt0"], ["gt1"])
                        act(t2, t1, AF.Sigmoid, ["gt1"], ["gt2"], scale=GC2)
                        tt("dve", hidT[:, hk, 0:127], xg, t2, ALU.mult, ["gt0", "gt2"], ["hidT"])
                    if which == 0:
                        for hk in range(2):
                            mm(PB(6)[0:64, 0:127], w2c[:, hk, :], hidT[:, hk, 0:127], hk == 0, hk == 1, ["w2c", "hidT"], [pk(6)])
                        cp("dve", kcmpT[0:64, 0:127], PB(6)[0:64, 0:127], [pk(6)], ["kcmpT"])
                    else:
                        for hk in range(2):
                            mm(PB(6)[0:127, 0:64], hidT[:, hk, 0:127], w2c[:, hk, :], hk == 0, hk == 1, ["w2c", "hidT"], [pk(6)])
                        cp("dve", vcA[0:127, 0:64], PB(6)[0:127, 0:64], [pk(6)], ["vcA"])

                sb_rr = [0]

                def sbank():
                    b = sb_rr[0] % 3
                    sb_rr[0] += 1
                    return b

                p_rr = [0]

                def pbuf():
                    i = p_rr[0] % NPT
                    p_rr[0] += 1
                    return i

                e_rr = [0]

                def ebuf():
                    i = e_rr[0] % 3
                    e_rr[0] += 1
                    return i

                def qsl(qt, rows):
                    return qT[0:rows, :, qt * 128:(qt + 1) * 128]

                def score_tile(lhsT, lkey, rows, qt, table, tkey, nk=128):
                    b = sbank()
                    qkeys = ["qT"] + ([f"qTm{qt}"] if rows > 64 else [])
                    ps3 = PB(b)[0:nk, :].rearrange("p (r q) -> p r q", r=4)
                    mm(ps3, lhsT, qsl(qt, rows), True, table is None, [lkey] + qkeys, [pk(b)])
                    if table is not None:
                        mm(ps3, ident[0:nk, 0:nk], table, False, True, ["ident", tkey], [pk(b)])
                    pi = pbuf()
                    act(Pt[pi][0:nk, :], PB(b)[0:nk, :], AF.Exp, [pk(b)], [f"P{pi}"], scale=0.125)
                    return pi

                jobs = []

                def jobC(qt):
                    op_ = qt % 2
                    nk = min(127, 8 * qt + 7)

                    def s1():
                        return score_tile(kcmpT[0:64, 0:nk], "kcmpT", 64, qt, Ec[0:nk, qt, :, :], "Ec", nk=nk)

                    def s2(pi):
                        acc = PB(3)[:, 0:388].rearrange("p (r c) -> p r c", r=4)
                        for r in range(4):
                            mm(acc[:, r, :], Pt[pi][0:nk, r * 128:(r + 1) * 128], vcA[0:nk, 0:97], True, True, [f"P{pi}", "vcA"], [pk(3)])
                        rden = sml[:, 0:4]
                        ts("dve", rden, acc[:, :, 64], 1e-30, None, ALU.max, None, [pk(3)], ["rdc"])
                        recip(rden, rden, ["rdc"], ["rdc"])
                        ts("dve", imp, acc[:, 0, 65:97], rden[:, 0:1], None, ALU.mult, None, [pk(3), "rdc"], ["imp"])
                        for r in range(1, 4):
                            stt(imp, acc[:, r, 65:97], rden[:, r:r + 1], imp, ALU.mult, ALU.add, [pk(3), "rdc", "imp"], ["imp"])
                        scc = sml[:, 4:8]
                        tt("dve", scc, rden, gsig[:, qt, :].rearrange("p (r c) -> p r c", r=4)[:, :, 0], ALU.mult, ["rdc", "gsig"], ["scc"])
                        tt("dve", otile[op_], acc[:, :, 0:64], scc.unsqueeze(2).to_broadcast([128, 4, 64]), ALU.mult, [pk(3), "scc"], [f"ot{op_}"])
                        tt("dve", imp, imp, cmfm[:, 0, qt, :], ALU.mult, ["imp", "cmfm"], ["imp"])
                        tt("dve", imp, imp, cmfm[:, 1, qt, :], ALU.add, ["imp", "cmfm"], ["imp"])
                        S.add("dve", lambda e: e.max(out=m8[:, 0:8], in_=imp), ["imp"], ["m8a"])
                        S.add("dve", lambda e: e.match_replace(out=impw, in_to_replace=m8[:, 0:8], in_values=imp, imm_value=-3.0e4), ["imp", "m8a"], ["impw"])
                        S.add("dve", lambda e: e.max(out=m8[:, 8:16], in_=impw), ["impw"], ["m8b"])
                        ts("dve", impw, imp, m8[:, 15:16], None, ALU.is_ge, None, ["imp", "m8b"], ["impw"])
                        ts("dve", negm[op_], impw, 1000.0, -1000.0, ALU.mult, ALU.add, ["impw"], [f"negm{op_}"])

                    jobs.append((s1, s2))

                def mask_T(qt):
                    op_ = qt % 2
                    ptn = PBb(3)[0:32, 896:1024]
                    tr(ptn, negm[op_], [f"negm{op_}"], [pk(3)])
                    cp("dve", qT[64:96, :, qt * 128:(qt + 1) * 128], ptn.unsqueeze(1).to_broadcast([32, 4, 128]), [pk(3)], [f"qTm{qt}"])

                def jobsKV(qt, kts, lhs_of, lkey, rows, vA, vkey, accb, tables, gcol, sc_off, pre=None, post=None):
                    op_ = qt % 2
                    acc = PB(accb)[:, 0:260].rearrange("p (r c) -> p r c", r=4)
                    for ii, kt in enumerate(kts):
                        first = ii == 0
                        last = ii == len(kts) - 1

                        def s1(kt=kt, first=first):
                            if first and pre is not None:
                                pre()
                            table, tkey = tables(kt)
                            return score_tile(lhs_of(kt), lkey, rows, qt, table, tkey)

                        def s2(pi, kt=kt, first=first, last=last):
                            if first:
                                mm(PB(accb)[:, 0:260], zer_b[:, 0:128], zer_b[:, 0:260], True, False, ["zer_b"], [pk(accb)], skip_group_check=True)
                            for r in range(4):
                                mm(acc[:, r, :], Pt[pi][:, r * 128:(r + 1) * 128], vA[:, kt, :], False, last, [f"P{pi}", vkey], [pk(accb)], skip_group_check=True)
                            if last:
                                rden = sml[:, sc_off:sc_off + 4]
                                kk = f"rd{sc_off}"
                                ts("dve", rden, acc[:, :, 64], 1e-30, None, ALU.max, None, [pk(accb)], [kk])
                                recip(rden, rden, [kk], [kk])
                                tt("dve", rden, rden, gsig[:, qt, :].rearrange("p (r c) -> p r c", r=4)[:, :, gcol], ALU.mult, [kk, "gsig"], [kk])
                                ot2 = ot2b[sc_off].rearrange("p (r d) -> p r d", r=4)
                                tt("dve", ot2, acc[:, :, 0:64], rden.unsqueeze(2).to_broadcast([128, 4, 64]), ALU.mult, [pk(accb), kk], ["ot2_" + str(sc_off)])
                                tt("dve", otile[op_], otile[op_], ot2, ALU.add, [f"ot{op_}", "ot2_" + str(sc_off)], [f"ot{op_}"])
                                if post is not None:
                                    post()

                        jobs.append((s1, s2))

                def en(d):
                    return enear[:, d, 4 * g:4 * g + 4, :]

                mwb = maskw4

                def tabW(qt):
                    def f(kt):
                        dl = qt - kt
                        if dl == 0:
                            return en(0), "enear"
                        if dl == 1:
                            return en(1), "enear"
                        if dl == 4:
                            return mwb, "maskw4"
                        return None, None
                    return f

                def tabS(qt):
                    def f(kt):
                        dl = qt - kt
                        if dl == 0:
                            return en(0), "enear"
                        if dl == 1:
                            return en(1), "enear"
                        return None, None
                    return f

                def out_dma(qt):
                    def f():
                        if qt + 1 < NTT:
                            mask_T(qt + 1)
                        dma("sp", onsa_s[sq, qt * 128:(qt + 1) * 128, g * 256:(g + 1) * 256], otile[qt % 2].rearrange("p r d -> p (r d)"), [f"ot{qt%2}"], ["onsa_s"])
                        pc = next(precast_it, None)
                        if pc is not None:
                            pc()
                    return f

                jobC(0)
                for qt in range(NTT):
                    if qt + 1 < NTT:
                        jobC(qt + 1)
                    ktsW = list(range(max(0, qt - 4), qt + 1))
                    jobsKV(qt, ktsW, lambda kt: kwT[0:64, kt * 128:(kt + 1) * 128], "kwT", 64, vwA, "vwA", 6 + qt % 2, tabW(qt), 2, 8)
                    ktsS = list(range(0, qt + 1))
                    jobsKV(qt, ktsS, lambda kt: ksT[0:96, kt * 128:(kt + 1) * 128], "ksT", 96, vsA, "vsA", 4 + qt % 2, tabS(qt), 1, 12,
                           pre=((lambda: mask_T(0)) if qt == 0 else None), post=out_dma(qt))
                LOOK = 2
                pis = {}
                for i in range(len(jobs) + LOOK):
                    if i < len(jobs):
                        pis[i] = jobs[i][0]()
                    j = i - LOOK
                    if j >= 0:
                        jobs[j][1](pis.pop(j))
            S.barrier()
            AR.reset(base_m)

            xt = [AR.f32(D) for _ in range(2)]
            junk = AR.bf(D)
            abf = [AR.bf(D) for _ in range(2)]
            ynT = [AR.bf(8 * 128).rearrange("p (k t) -> p k t", k=8) for _ in range(2)]
            for t in range(NTT):
                par = t % 2
                dma("sp", xt[par][:, 0:1024], onsa_s[sq, t * 128:(t + 1) * 128, :], ["onsa_s"], [f"xt{par}"])
                rms_to_T(xt[par][:, 0:1024], f"xt{par}", ynT[par], f"ynT{par}", 0, V_GNSA, par, 3 * par, nk=8)
                dma("sp", yT_s[sq, :, 8:16, t * 128:(t + 1) * 128], ynT[par], [f"ynT{par}"], ["yT_s"])
            S.barrier()
            AR.reset(0)
            if stop_after == "mixer":
                continue
            for pc in precast_it:
                pc()

            hT = AR.f32(4 * D).rearrange("p (t c) -> p t c", t=4)
            tmpT = AR.f32(4 * D).rearrange("p (t c) -> p t c", t=4)
            actT = AR.bf(16 * 512).rearrange("p (k t) -> p k t", k=16)
            hid = AR.bf(64 * 512).rearrange("p (k t) -> p k t", k=64)
            wbuf = [AR.bf(4096) for _ in range(3)]
            gr1 = AR.f32(D)
            abf = [AR.bf(D) for _ in range(2)]
            ptile = AR.f32(256)
            pb16 = AR.bf(256)
            pTt = AR.bf(2 * 512).rearrange("p (k t) -> p k t", k=2)
            rtmp = [AR.f32(512) for _ in range(2)]
            ssqp = AR.f32(64)
            wb_rr = [0]

            def next_wbuf():
                wi = wb_rr[0] % 3
                wb_rr[0] += 1
                return wi

            def gemm_tok(ws_src, wkey, nkc, lhs_key, lhs_of, evac, hook=None):
                for half in range(2):
                    for k4 in range(0, nkc, 4):
                        nkk = min(4, nkc - k4)
                        wi = next_wbuf()
                        wt_ = wbuf[wi][:, 0:nkk * 1024].rearrange("p (k c) -> p k c", k=nkk)
                        dma("sp", wt_, ws_src[k4 * 128:(k4 + nkk) * 128, half * 1024:(half + 1) * 1024].rearrange("(k p) c -> p k c", p=128), [wkey], [f"wbuf{wi}"])
                        for kk in range(nkk):
                            kc = k4 + kk
                            for t4 in range(4):
                                for cb in range(2):
                                    b = t4 * 2 + cb
                                    mm(PB(b), lhs_of(kc, t4), wt_[:, kk, cb * 512:(cb + 1) * 512], kc == 0, kc == nkc - 1, [lhs_key, f"wbuf{wi}"], [pk(b)])
                        if hook is not None and half == 0 and k4 == 0:
                            hook()
                    for t4 in range(4):
                        for cb in range(2):
                            evac(half, t4, cb, PB(t4 * 2 + cb), t4 * 2 + cb)

            def evac_norm(half, t4, cb, ps, b):
                col = slice(half * 1024 + cb * 512, half * 1024 + (cb + 1) * 512)
                idx = t4 * 4 + half * 2 + cb
                if b % 2 == 0:
                    act(tmpT[:, t4, col], ps, AF.Copy, [pk(b)], [f"tmpT{t4}"])
                else:
                    cp("dve", tmpT[:, t4, col], ps, [pk(b)], [f"tmpT{t4}"])
                stt(rtmp[b % 2], tmpT[:, t4, col], 1.0, tmpT[:, t4, col], ALU.mult, ALU.mult, [f"tmpT{t4}"], [f"rtmp{b%2}", f"ssqp{idx}"],
                    accum_out=ssqp[:, idx:idx + 1])
                tt("pool", tmpT[:, t4, col], tmpT[:, t4, col], gr1[:, col], ALU.mult, [f"tmpT{t4}", "gr1"], [f"tmpT{t4}"])

            def post_norm_add(t4, st_off):
                ssq = stats[:, st_off:st_off + 1]
                kk = f"pn{st_off}"
                S.add("dve", lambda e: e.reduce_sum(out=ssq, in_=ssqp[:, t4 * 4:(t4 + 1) * 4], axis=mybir.AxisListType.X),
                      [f"ssqp{t4*4+i}" for i in range(4)], [kk])
                act(ssq, ssq, AF.Ln, [kk], [kk], bias=EPS, scale=1.0 / D)
                act(ssq, ssq, AF.Exp, [kk], [kk], scale=-0.5)
                stt(hT[:, t4, :], tmpT[:, t4, :], ssq, hT[:, t4, :], ALU.mult, ALU.add, [f"tmpT{t4}", kk, f"hT{t4}"], [f"hT{t4}"])

            for blk in range(4):
                tok0 = blk * 512
                if blk == 0:
                    dma("sp", hid[:, 0:16, :], yT_s[sq, :, :, tok0:tok0 + 512], ["yT_s"], ["hid"])

                def xload(tok0=tok0):
                    dma("sp", gr1, grow[:, 0, :], (), ["gr1"])
                    for t4 in range(4):
                        dma("sp", hT[:, t4, :], x[sq, tok0 + t4 * 128: tok0 + (t4 + 1) * 128, :], (), [f"hT{t4}"])

                gemm_tok(ws_out, "ws_out", 16, "hid", lambda kc, t4: hid[:, kc, t4 * 128:(t4 + 1) * 128], evac_norm, hook=xload)
                for t4 in range(4):
                    post_norm_add(t4, 6 + t4)
                dma("sp", gr1, grow[:, 1, :], (), ["gr1"])
                for t4 in range(4):
                    rms_to_T(hT[:, t4, :], f"hT{t4}", actT, "actT", t4 * 128, V_GMLP, t4 % 2, 10 + 3 * (t4 % 2), pbs=(6, 7),
                             junk_ap=tmpT[:, t4, 0:1024].bitcast(BF16), junk_key=f"tmpT{t4}", scale_on_act=True)
                for h2 in range(32):
                    wi = next_wbuf()
                    wt_ = wbuf[wi].rearrange("p (k c) -> p k c", k=16)
                    dma("sp", wt_, ws_1[:, h2 * 256:(h2 + 1) * 256].rearrange("(k p) c -> p k c", p=128), ["ws_1"], [f"wbuf{wi}"])
                    for j in range(2):
                        hc = h2 * 2 + j
                        b = hc % 4
                        for kc in range(16):
                            mm(PB(b), wt_[:, kc, j * 128:(j + 1) * 128], actT[:, kc, :], kc == 0, kc == 15, [f"wbuf{wi}", "actT"], [pk(b)])
                        rt = rtmp[hc % 2]
                        act(rt, PB(b), AF.Relu, [pk(b)], [f"rtmp{hc%2}"])
                        tt("pool", hid[:, hc, :], rt, rt, ALU.mult, [f"rtmp{hc%2}"], ["hid"])
                gemm_tok(ws_2, "ws_2", 64, "hid", lambda kc, t4: hid[:, kc, t4 * 128:(t4 + 1) * 128], evac_norm)
                if blk + 1 < 4:
                    dma("sp", hid[:, 0:16, :], yT_s[sq, :, :, tok0 + 512:tok0 + 1024], ["yT_s"], ["hid"])
                for t4 in range(4):
                    post_norm_add(t4, 6 + t4)
                for t4 in range(4):
                    ab = abf[t4 % 2]
                    abk = f"abf{t4%2}"
                    act(ab, hT[:, t4, :], AF.Copy, [f"hT{t4}"], [abk])
                    for k4 in range(4):
                        b = 6 + k4 % 2
                        pt = PBb(b)[:, 0:512].rearrange("p (j q) -> p j q", j=4)
                        for j in range(4):
                            kc = k4 * 4 + j
                            tr(pt[:, j, :], ab[:, kc * 128:(kc + 1) * 128], [abk], [pk(b)])
                        cp("dve", actT[:, k4 * 4:(k4 + 1) * 4, t4 * 128:(t4 + 1) * 128], pt, [pk(b)], ["actT"])
                    dma("sp", ptile, pin[sq, tok0 + t4 * 128: tok0 + (t4 + 1) * 128, :], (), ["ptile"])
                    cp("pool", pb16, ptile, ["ptile"], ["pb16"])
                    pt = PBb(5)[:, 0:256].rearrange("p (j q) -> p j q", j=2)
                    for j in range(2):
                        tr(pt[:, j, :], pb16[:, j * 128:(j + 1) * 128], ["pb16"], [pk(5)])
                    cp("dve", pTt[:, :, t4 * 128:(t4 + 1) * 128], pt, [pk(5)], ["pTt"])

                def evac_sig(half, t4, cb, ps, b):
                    col = slice(half * 1024 + cb * 512, half * 1024 + (cb + 1) * 512)
                    act(tmpT[:, t4, col], ps, AF.Sigmoid, [pk(b)], [f"tmpT{t4}"])

                gemm_tok(ws_g, "ws_g", 16, "actT", lambda kc, t4: actT[:, kc, t4 * 128:(t4 + 1) * 128], evac_sig)
                for half in range(2):
                    wi = next_wbuf()
                    wpT = wbuf[wi][:, 0:2048].rearrange("p (k c) -> p k c", k=2)
                    dma("sp", wpT, ws_p[:, half * 1024:(half + 1) * 1024].rearrange("(k p) c -> p k c", p=128), ["ws_p"], [f"wbuf{wi}"])
                    for kc in range(2):
                        for t4 in range(4):
                            for cb in range(2):
                                b = t4 * 2 + cb
                                mm(PB(b), pTt[:, kc, t4 * 128:(t4 + 1) * 128], wpT[:, kc, cb * 512:(cb + 1) * 512], kc == 0, kc == 1, ["pTt", f"wbuf{wi}"], [pk(b)])
                    for t4 in range(4):
                        for cb in range(2):
                            b = t4 * 2 + cb
                            col = slice(half * 1024 + cb * 512, half * 1024 + (cb + 1) * 512)
                            tt("dve", tmpT[:, t4, col], tmpT[:, t4, col], PB(b), ALU.mult, [f"tmpT{t4}", pk(b)], [f"tmpT{t4}"])
                for t4 in range(4):
                    tt("pool", tmpT[:, t4, :], tmpT[:, t4, :], hT[:, t4, :], ALU.add, [f"tmpT{t4}", f"hT{t4}"], [f"tmpT{t4}"])
                    dma("pool", out[sq, tok0 + t4 * 128: tok0 + (t4 + 1) * 128, :], tmpT[:, t4, :], [f"tmpT{t4}"], ["out"])

        S.barrier(engines=("sp",))
        S.emit(st)
    return nc, S


def _bucket(dist):
    n = np.maximum(dist, 0)
    nf = np.maximum(n, 1).astype(np.float32)
    large = 16 + (np.log(nf / np.float32(16)) / np.float32(math.log(128 / 16)) * np.float32(16)).astype(np.int32)
    large = np.minimum(large, 31)
    return np.where(n < 16, n, large)


def _pm(v, n):
    return np.ascontiguousarray(np.asarray(v, np.float32).reshape(n, 128).T)


def _host_consts(inp):
    f = lambda k: np.asarray(inp[k], np.float32)
    rel = f("rel_bias")
    vecs = np.zeros((128, NVEC), np.float32)
    vecs[:, V_GPRE:V_GPRE + 16] = _pm(f("norm_mix_pre")[0], 16)
    vecs[:, V_GMLP:V_GMLP + 16] = _pm(f("norm_mlp_pre")[0], 16)
    vecs[:, V_GLRU:V_GLRU + 8] = _pm(f("gnorm_lru")[0], 8)
    vecs[:, V_GNSA:V_GNSA + 8] = _pm(f("gnorm_nsa")[0], 8)
    cw = f("conv_w")[0]
    vecs[:, V_CW:V_CW + 32] = cw.reshape(4, 8, 128).transpose(2, 1, 0).reshape(128, 32)
    vecs[:, V_CB:V_CB + 8] = _pm(f("conv_b")[0], 8)
    vecs[:, V_BA:V_BA + 8] = _pm(f("lru_ba")[0].reshape(-1), 8)
    vecs[:, V_BX:V_BX + 8] = _pm(f("lru_bx")[0].reshape(-1), 8)
    vecs[:, V_LAM:V_LAM + 8] = _pm(f("lru_lambda")[0], 8)
    vecs[:, V_C31:V_C31 + 16] = np.broadcast_to(rel[31][None, :], (128, 16))
    grow = np.ascontiguousarray(np.broadcast_to(np.stack([f("norm_mix_post")[0], f("norm_mlp_post")[0]])[None], (128, 2, D)))
    bdm = np.zeros((128, 2, 8, 128), np.float32)
    for a_, key in enumerate(("lru_wa", "lru_wx")):
        wmat = f(key)[0]
        for c in range(8):
            bdm[0:64, a_, c, 0:64] = wmat[2 * c]
            bdm[64:128, a_, c, 64:128] = wmat[2 * c + 1]
    k = np.arange(128)[:, None, None]
    dd = np.arange(2)[None, :, None]
    q = np.arange(128)[None, None, :]
    dist = q + 128 * dd - k
    tb = rel[_bucket(dist)]
    tb = np.where((dist >= 0)[..., None], tb, np.float32(-30000.0)).transpose(0, 1, 3, 2)
    tb_near = np.ascontiguousarray(tb.astype(np.float32))
    maskw = np.where(np.arange(128)[None, :] < np.arange(128)[:, None], 0.0, -240000.0).astype(ml_dtypes.bfloat16)
    n = np.arange(127)[:, None, None]
    qt = np.arange(16)[None, :, None]
    i = np.arange(128)[None, None, :]
    dc = 128 * qt + i - 16 * n - 31
    tbc = rel[_bucket(dc)]
    tbc = np.where((dc >= 0)[..., None], tbc, np.float32(-30000.0))
    tbc = tbc.reshape(127, 16, 128, 4, 4).transpose(3, 0, 1, 4, 2)
    tb_c = np.full((4, 128, 16, 4, 128), -30000.0, np.float32)
    tb_c[:, 0:127] = tbc
    ii = np.arange(128)[:, None, None]
    qq = np.arange(16)[None, :, None]
    jj = np.arange(32)[None, None, :]
    dblk = (128 * qq + ii) // 64 - jj
    forced = (jj == 0) | ((dblk >= 0) & (dblk < 2))
    causal = dblk >= 0
    cm = (causal & ~forced).astype(np.float32)
    fmv = np.where(forced, np.float32(1e4), np.where(causal, np.float32(0), np.float32(-1e4))).astype(np.float32)
    cmfm = np.ascontiguousarray(np.stack([cm, fmv], axis=1))
    ex = (np.arange(T)[None, :] // 64 == np.arange(32)[:, None]).astype(ml_dtypes.bfloat16)
    n_cmp, n_sel = 127, 32
    jjn = np.arange(n_sel)[:, None, None]
    ci = 4 * jjn + np.arange(4)[None, :, None] - np.arange(2)[None, None, :]
    jb = np.broadcast_to(jjn, ci.shape)
    ok = (ci >= 0) & (ci < n_cmp)
    Mm = np.zeros((n_cmp, n_sel), np.float32)
    np.add.at(Mm, (ci[ok], jb[ok]), 1.0)
    wi = f("w_in")[0]
    w_in_g = np.ascontiguousarray(np.stack([np.concatenate(
        [wi[:, IN_OFF["q"] + g * 256: IN_OFF["q"] + (g + 1) * 256]]
        + [wi[:, IN_OFF[nm] + g * 64: IN_OFF[nm] + (g + 1) * 64] for nm in ("kc", "vc", "ks", "kw", "vs", "vw")]
        + [wi[:, IN_OFF["gt"] + g * 12: IN_OFF["gt"] + (g + 1) * 12]], axis=1) for g in range(4)]))
    w_in_l = np.ascontiguousarray(np.stack([np.concatenate(
        [wi[:, c * 128:(c + 1) * 128], wi[:, 1024 + c * 128: 1024 + (c + 1) * 128]], axis=1) for c in range(8)]))
    return dict(
        vecs=vecs, grow=grow, bd=bdm, ident=np.eye(128, dtype=ml_dtypes.bfloat16), tb_near=tb_near, maskw=maskw,
        tb_c=tb_c, cmfm=cmfm, ex=ex, Mmat=Mm.astype(ml_dtypes.bfloat16),
        pekT=np.ascontiguousarray(f("cmp_pe_k")[0].T), pevT=np.ascontiguousarray(f("cmp_pe_v")[0].T),
        w_in_g=w_in_g, w_in_l=w_in_l, w_out=f("w_out")[0], mlp_w1=f("mlp_w1")[0], mlp_w2=f("mlp_w2")[0],
        ple_gate=f("ple_gate")[0], ple_proj=f("ple_proj")[0],
        cmp_w1_k=f("cmp_w1_k")[0], cmp_w1_v=f("cmp_w1_v")[0], cmp_w2_k=f("cmp_w2_k")[0], cmp_w2_v=f("cmp_w2_v")[0],
    )


_CACHE = {}


def kernel(**inputs):
    consts = _host_consts(inputs)
    x = np.asarray(inputs["x"], np.float32)
    p = np.asarray(inputs["p"], np.float32)
    n = 8
    if "nc" not in _CACHE:
        _CACHE["nc"] = build(nseq=2)[0]
    nc = _CACHE["nc"]
    in_maps = []
    for c in range(n):
        m = dict(consts)
        m["x"] = np.ascontiguousarray(x[2 * c:2 * c + 2])
        m["p"] = np.ascontiguousarray(p[0, 2 * c:2 * c + 2])
        in_maps.append(m)
    res = run_bass_kernel_spmd(nc, in_maps, core_ids=list(range(n)))
    return np.concatenate([np.asarray(r["out"], np.float32) for r in res.results], axis=0)
```

```python
from contextlib import ExitStack
import math
import numpy as np
import ml_dtypes
import concourse.bass as bass
import concourse.mybir as mybir
from concourse.bass_utils import run_bass_kernel_spmd

F32 = mybir.dt.float32
BF16 = mybir.dt.bfloat16
AF = mybir.ActivationFunctionType
ALU = mybir.AluOpType

ENGS = ("pe", "act", "dve", "pool", "sp")
T = 2048
D = 2048
NTT = 16
EPS = 1e-6
GC1 = 0.044715
GC2 = 1.5957691216057308


class Sched:
    def __init__(self, nc):
        self.nc = nc
        self.streams = {e: [] for e in ENGS}
        self.count = {e: 0 for e in ENGS}
        self.last_w = {}
        self.readers = {}
        self.waited = {e: {} for e in ENGS}
        self.nd = {"sp": 14, "pool": 8}
        self.dma_rr = {e: 0 for e in self.nd}
        self.dma_cnt = {e: [0] * self.nd[e] for e in self.nd}
        self.sems = {}
        self.n_instr = 0

    @staticmethod
    def _ev_sem(ev):
        if ev[0] == "c":
            return ("c", ev[1]), ev[2]
        return ("d", ev[1], ev[2]), ev[3]

    def add(self, eng, fn, reads=(), writes=(), dma=False):
        deps = set()
        for k in reads:
            for w_ in self.last_w.get(k, ()):
                deps.add(w_)
            if isinstance(k, str) and k.startswith("pb") and k[2:].isdigit():
                for r in self.readers.get(k, ()):
                    if not (r[0] == "c" and r[1] == eng):
                        deps.add(r)
        for k in writes:
            for w_ in self.last_w.get(k, ()):
                if dma and w_[0] == "d":
                    continue
                deps.add(w_)
            for r in self.readers.get(k, ()):
                deps.add(r)
        need = {}
        for ev in deps:
            if ev[0] == "c" and ev[1] == eng and eng == "pe":
                continue
            s, v = self._ev_sem(ev)
            if need.get(s, 0) < v:
                need[s] = v
        if dma:
            idx = self.dma_rr[eng]
            self.dma_rr[eng] = (idx + 1) % self.nd[eng]
            prev = self.dma_cnt[eng][idx]
            self.dma_cnt[eng][idx] = prev + 16
            if prev > 0:
                s = ("d", eng, idx)
                if need.get(s, 0) < prev:
                    need[s] = prev
            myev = ("d", eng, idx, prev + 16)
            inc = (("d", eng, idx), 16)
        else:
            self.count[eng] += 1
            myev = ("c", eng, self.count[eng])
            inc = (("c", eng), 1)
        waits = []
        wd = self.waited[eng]
        for s, v in need.items():
            if wd.get(s, 0) >= v:
                continue
            wd[s] = v
            waits.append((s, v))
        self.streams[eng].append((waits, fn, inc))
        self.n_instr += 1
        for k in reads:
            self.readers.setdefault(k, []).append(myev)
        for k in writes:
            prevw = self.last_w.get(k, [])
            if dma and prevw and prevw[0][0] == "d" and not self.readers.get(k):
                self.last_w[k] = prevw + [myev]
            else:
                self.last_w[k] = [myev]
            self.readers[k] = []
        return myev

    def _all_now(self):
        cur = []
        for e in ENGS:
            if self.count[e] > 0:
                cur.append((("c", e), self.count[e]))
        for e in self.nd:
            for i in range(self.nd[e]):
                if self.dma_cnt[e][i] > 0:
                    cur.append((("d", e, i), self.dma_cnt[e][i]))
        return cur

    def barrier(self, engines=ENGS):
        cur = self._all_now()
        for e in engines:
            wd = self.waited[e]
            waits = []
            for s, v in cur:
                if wd.get(s, 0) >= v:
                    continue
                wd[s] = v
                waits.append((s, v))
            if waits:
                self.streams[e].append((waits, None, None))
        self.last_w = {}
        self.readers = {}

    def emit(self, stack):
        nc = self.nc
        semkeys = [("c", e) for e in ENGS]
        for e in self.nd:
            for i in range(self.nd[e]):
                semkeys.append(("d", e, i))
        for k in semkeys:
            self.sems[k] = stack.enter_context(nc.semaphore("s_" + "_".join(str(x) for x in k)))
        block = stack.enter_context(nc.Block())
        sems = self.sems

        def run(engobj, stream):
            for waits, fn, inc in stream:
                for s, v in waits:
                    engobj.wait_ge(sems[s], v)
                if fn is None:
                    continue
                ins = fn(engobj)
                ins.then_inc(sems[inc[0]], inc[1])

        @block.tensor
        def _(e):
            run(e, self.streams["pe"])

        @block.scalar
        def _(e):
            run(e, self.streams["act"])

        @block.vector
        def _(e):
            run(e, self.streams["dve"])

        @block.gpsimd
        def _(e):
            run(e, self.streams["pool"])

        @block.sync
        def _(e):
            run(e, self.streams["sp"])


class Arena:
    def __init__(self, t, words):
        self.t = t
        self.words = words
        self.off = 0

    def f32(self, n):
        assert self.off + n <= self.words, ("arena overflow", self.off, n)
        v = self.t[:, self.off:self.off + n]
        self.off += n
        return v

    def bf(self, n):
        w = (n + 1) // 2
        assert self.off + w <= self.words, ("arena overflow", self.off, w)
        v = self.t[:, self.off:self.off + w].bitcast(BF16)
        self.off += w
        return v

    def mark(self):
        return self.off

    def reset(self, m):
        self.off = m


V_GPRE, V_GMLP, V_GLRU, V_GNSA, V_CW, V_CB, V_BA, V_BX, V_LAM, V_C31 = 0, 16, 32, 40, 48, 80, 88, 96, 104, 112
NVEC = 128

IN_OFF = dict(u=0, g=1024, q=2048, kc=3072, vc=3328, ks=3584, vs=3840, kw=4096, vw=4352, gt=4608)


def build(nseq=2, dbg=False, stop_after=None):
    nc = bass.Bass("TRN2", target_bir_lowering=False)

    def din(name, shape, dt=F32):
        return nc.dram_tensor(name, list(shape), dt, kind="ExternalInput").ap()

    x = din("x", [nseq, T, D])
    pin = din("p", [nseq, T, 256])
    w_in_g = din("w_in_g", [4, D, 652])
    w_in_l = din("w_in_l", [8, D, 256])
    w_out = din("w_out", [D, D])
    w1 = din("mlp_w1", [D, 8192])
    w2 = din("mlp_w2", [8192, D])
    wg = din("ple_gate", [D, D])
    wp = din("ple_proj", [256, D])
    cw1k = din("cmp_w1_k", [2048, 256])
    cw1v = din("cmp_w1_v", [2048, 256])
    cw2k = din("cmp_w2_k", [256, 64])
    cw2v = din("cmp_w2_v", [256, 64])
    pekT = din("pekT", [64, 32])
    pevT = din("pevT", [64, 32])
    vecs = din("vecs", [128, NVEC])
    grow = din("grow", [128, 2, D])
    bd = din("bd", [128, 2, 8, 128])
    ident_d = din("ident", [128, 128], BF16)
    tbn = din("tb_near", [128, 2, 16, 128])
    maskw_d = din("maskw", [128, 128], BF16)
    tbc = din("tb_c", [4, 128, 16, 4, 128])
    cmfm_d = din("cmfm", [128, 2, 16, 32])
    ex_d = din("ex", [32, T], BF16)
    M_d = din("Mmat", [127, 32], BF16)
    out = nc.dram_tensor("out", [nseq, T, D], F32, kind="ExternalOutput").ap()

    ws_out = nc.dram_tensor("ws_out", [D, D], BF16).ap()
    ws_1 = nc.dram_tensor("ws_1", [D, 8192], BF16).ap()
    ws_2 = nc.dram_tensor("ws_2", [8192, D], BF16).ap()
    ws_g = nc.dram_tensor("ws_g", [D, D], BF16).ap()
    ws_p = nc.dram_tensor("ws_p", [256, D], BF16).ap()
    ws_c1 = nc.dram_tensor("ws_c1", [2, 64, 32, 256], BF16).ap()
    if dbg:
        yT_s = nc.dram_tensor("yT_s", [nseq, 128, 16, T], BF16, kind="ExternalOutput").ap()
        onsa_s = nc.dram_tensor("onsa_s", [nseq, T, 1024], F32, kind="ExternalOutput").ap()
    else:
        yT_s = nc.dram_tensor("yT_s", [nseq, 128, 16, T], BF16).ap()
        onsa_s = nc.dram_tensor("onsa_s", [nseq, T, 1024], F32).ap()

    S = Sched(nc)

    with ExitStack() as st:
        AW = 49200
        arena_t = st.enter_context(nc.sbuf_tensor("arena", [128, AW], F32))
        cst_t = st.enter_context(nc.sbuf_tensor("cst", [128, 3968], F32))
        AR = Arena(arena_t, AW)
        CS = Arena(cst_t, 3968)
        pbank = [st.enter_context(nc.psum_tensor(f"pb{i}", [128, 512], F32)) for i in range(8)]

        def PB(i):
            return pbank[i][:]

        def PBb(i):
            return pbank[i][:].bitcast(BF16)

        def pk(i):
            return f"pb{i}"

        def mm(o, lhsT, rhs, start, stop, r, w, **kw):
            S.add("pe", lambda e: e.matmul(o, lhsT=lhsT, rhs=rhs, start=start, stop=stop, **kw), r, w)

        def tr(o, in_, r, w):
            S.add("pe", lambda e: e.transpose(out=o, in_=in_, identity=ident[:, :]), list(r) + ["ident"], w)

        def act(o, in_, func, r, w, **kw):
            S.add("act", lambda e: e.activation(out=o, in_=in_, func=func, **kw), r, w)

        def tt(eng, o, a, b, op, r, w):
            S.add(eng, lambda e: e.tensor_tensor(out=o, in0=a, in1=b, op=op), r, w)

        def ts(eng, o, a, s1, s2, op0, op1, r, w, **kw):
            if s2 is None:
                S.add(eng, lambda e: e.tensor_scalar(out=o, in0=a, scalar1=s1, scalar2=None, op0=op0, **kw), r, w)
            else:
                S.add(eng, lambda e: e.tensor_scalar(out=o, in0=a, scalar1=s1, scalar2=s2, op0=op0, op1=op1, **kw), r, w)

        def stt(o, a, sc, b, op0, op1, r, w, **kw):
            S.add("dve", lambda e: e.scalar_tensor_tensor(out=o, in0=a, scalar=sc, in1=b, op0=op0, op1=op1, **kw), r, w)

        def cp(eng, o, a, r, w):
            S.add(eng, lambda e: e.tensor_copy(out=o, in_=a), r, w)

        def memset(eng, o, val, w):
            S.add(eng, lambda e: e.memset(o, val), (), w)

        def recip(o, a, r, w):
            S.add("dve", lambda e: e.reciprocal(out=o, in_=a), r, w)

        def dma(q, o, in_, r, w):
            return S.add(q, lambda e: e.dma_start(out=o, in_=in_), r, w, dma=True)

        ident = CS.bf(128)
        maskw = CS.bf(128)
        vec = CS.f32(NVEC)
        negc = CS.f32(16)
        cl = CS.f32(8)
        cltmp = CS.f32(8)
        enear = CS.bf(2 * 16 * 128).rearrange("p (d h q) -> p d h q", d=2, h=16)
        cmfm = CS.f32(2 * 16 * 32).rearrange("p (a t j) -> p a t j", a=2, t=16)
        ones_f = CS.f32(128)
        zer_b = CS.bf(264)
        maskw4 = CS.bf(512).rearrange("p (r q) -> p r q", r=4)
        neg8c = CS.f32(16)
        nbab = CS.f32(16)
        stats = CS.f32(32)
        dma("sp", ident, ident_d, (), ["ident"])
        dma("sp", maskw, maskw_d, (), ["maskw"])
        dma("sp", vec, vecs, (), ["vec"])
        dma("sp", cmfm, cmfm_d, (), ["cmfm"])
        memset("dve", ones_f, 1.0, ["ones_f"])
        memset("dve", zer_b, 0.0, ["zer_b"])
        ts("dve", negc, vec[:, V_C31:V_C31 + 16], -1.0, None, ALU.mult, None, ["vec"], ["negc"])
        ts("dve", neg8c, vec[:, V_C31:V_C31 + 16], -8.0, None, ALU.mult, None, ["vec"], ["neg8c"])
        ts("dve", nbab, vec[:, V_BA:V_BA + 16], -1.0, None, ALU.mult, None, ["vec"], ["nbab"])
        cp("dve", maskw4, maskw.unsqueeze(1).to_broadcast([128, 4, 128]), ["maskw"], ["maskw4"])
        act(cltmp, vec[:, V_LAM:V_LAM + 8], AF.Exp, ["vec"], ["cltmp"], scale=-1.0)
        act(cltmp, cltmp, AF.Ln, ["cltmp"], ["cltmp"], bias=1.0, scale=1.0)
        ts("dve", cl, cltmp, -8.0, None, ALU.mult, None, ["cltmp"], ["cl"])
        m0 = AR.mark()
        stg = AR.f32(2 * 16 * 128).rearrange("p (d h q) -> p d h q", d=2, h=16)
        dma("sp", stg, tbn, (), ["stg"])
        for h in range(16):
            act(enear[:, :, h, :], stg[:, :, h, :], AF.Identity, ["stg", "neg8c"], ["enear"], bias=neg8c[:, h:h + 1], scale=8.0)
        S.barrier()
        AR.reset(m0)

        dma("pool", ws_c1[0], cw1k.rearrange("(l d) h -> d l h", d=64), (), ["ws_c1"])
        dma("pool", ws_c1[1], cw1v.rearrange("(l d) h -> d l h", d=64), (), ["ws_c1"])
        precast_list = []

        def precast(dst, src, rows, step, key):
            for r0 in range(0, rows, step):
                precast_list.append(lambda r0=r0: dma("pool", dst[r0:r0 + step, :], src[r0:r0 + step, :], (), [key]))

        precast(ws_out, w_out, D, 512, "ws_out")
        precast(ws_g, wg, D, 512, "ws_g")
        precast(ws_p, wp, 256, 256, "ws_p")
        precast(ws_1, w1, D, 128, "ws_1")
        precast(ws_2, w2, 8192, 512, "ws_2")
        precast_it = iter(precast_list)

        for sq in range(nseq):
            S.barrier()
            AR.reset(0)
            aT = AR.bf(16 * T).rearrange("p (k t) -> p k t", k=16)
            base_m = AR.mark()

            xt = [AR.f32(D) for _ in range(2)]
            junk = AR.bf(D)
            abf = [AR.bf(D) for _ in range(2)]

            def rms_to_T(src_tile, src_key, dstT, dst_key, tok0, gcol, par, st_off, pbs=(0, 1), nk=16, junk_ap=None, junk_key="junk", scale_on_act=False):
                ssq = stats[:, st_off:st_off + 1]
                std = stats[:, st_off + 1:st_off + 2]
                rstd = stats[:, st_off + 2:st_off + 3]
                kk = f"st{st_off}"
                jk = junk[:, 0:nk * 128] if junk_ap is None else junk_ap
                act(jk, src_tile, AF.Square, [src_key], [junk_key, kk + "a"], accum_out=ssq)
                act(std, ssq, AF.Ln, [kk + "a"], [kk + "b"], bias=EPS, scale=1.0 / (nk * 128))
                act(rstd, std, AF.Exp, [kk + "b"], [kk + "c"], scale=-0.5)
                ab = abf[par]
                if scale_on_act:
                    act(ab[:, 0:nk * 128], src_tile, AF.Copy, [src_key, kk + "c"], [f"abf{par}"], scale=rstd)
                else:
                    ts("dve", ab[:, 0:nk * 128], src_tile, rstd, None, ALU.mult, None, [src_key, kk + "c"], [f"abf{par}"])
                for k4 in range(nk // 4):
                    b = pbs[k4 % 2]
                    pt = PBb(b)[:, 0:512].rearrange("p (j q) -> p j q", j=4)
                    for j in range(4):
                        kc = k4 * 4 + j
                        tr(pt[:, j, :], ab[:, kc * 128:(kc + 1) * 128], [f"abf{par}"], [pk(b)])
                    if gcol is None:
                        cp("dve", dstT[:, k4 * 4:(k4 + 1) * 4, tok0:tok0 + 128], pt, [pk(b)], [dst_key])
                    else:
                        gv = vec[:, gcol + k4 * 4:gcol + (k4 + 1) * 4].unsqueeze(2).to_broadcast([128, 4, 128])
                        tt("dve", dstT[:, k4 * 4:(k4 + 1) * 4, tok0:tok0 + 128], pt, gv, ALU.mult, [pk(b), "vec"], [dst_key])

            for t in range(NTT):
                par = t % 2
                dma("sp", xt[par], x[sq, t * 128:(t + 1) * 128, :], (), [f"xt{par}"])
                rms_to_T(xt[par], f"xt{par}", aT, "aT", t * 128, V_GPRE, par, 3 * par)
            S.barrier()
            AR.reset(base_m)

            ylru = AR.bf(8 * T).rearrange("p (c t) -> p c t", c=8)
            bdt = AR.f32(2 * 8 * 128).rearrange("p (a c m) -> p a c m", a=2, c=8)
            dma("sp", bdt, bd, (), ["bdt"])
            wl = [AR.bf(16 * 2 * 128).rearrange("p (k a c) -> p k a c", k=16, a=2) for _ in range(2)]
            upad = [AR.f32(3 + T) for _ in range(2)]
            NTMP = 9
            m_tmp = AR.mark()
            tmp = [[AR.f32(512) for _ in range(NTMP)] for _ in range(2)]
            hbuf = [AR.f32(512) for _ in range(2)]
            ysq_ring = [AR.bf(512) for _ in range(4)]
            ones_b = AR.bf(128)
            memset("pool", ones_b, 1.0, ["ones_b"])

            def lru_wload(c):
                dma("pool", wl[c % 2], w_in_l[c].rearrange("(k p) (a c) -> p k a c", p=128, a=2), (), [f"wl{c%2}"])

            def lru_s1(c, tp):
                wb, wk, up, uk = wl[c % 2], f"wl{c%2}", upad[c % 2], f"upad{c%2}"
                pr = (c * 4 + tp) % 2
                X = tmp[pr]
                tk = lambda i: f"tmp{pr}_{i}"
                tok = slice(tp * 512, (tp + 1) * 512)
                if tp == 0:
                    if c + 1 < 8:
                        lru_wload(c + 1)
                    memset("pool", up[:, 0:3], 0.0, [uk])
                for kc in range(16):
                    mm(PB(0), wb[:, kc, 0, :], aT[:, kc, tok], kc == 0, kc == 15, [wk, "aT"], [pk(0)])
                cp("dve", up[:, 3 + tp * 512: 3 + (tp + 1) * 512], PB(0), [pk(0)], [uk])
                for kc in range(16):
                    mm(PB(1), wb[:, kc, 1, :], aT[:, kc, tok], kc == 0, kc == 15, [wk, "aT"], [pk(1)])
                gg, t1 = X[7], X[8]
                act(gg, PB(1), AF.Copy, [pk(1)], [tk(7)])
                act(t1, PB(1), AF.Square, [pk(1)], [tk(8)])
                xc = X[0]
                cwc = lambda k: vec[:, V_CW + c * 4 + k: V_CW + c * 4 + k + 1]
                ts("dve", xc, up[:, tp * 512: tp * 512 + 512], cwc(0), vec[:, V_CB + c:V_CB + c + 1], ALU.mult, ALU.add, [uk, "vec"], [tk(0)])
                for k in range(1, 4):
                    stt(xc, up[:, tp * 512 + k: tp * 512 + k + 512], cwc(k), xc, ALU.mult, ALU.add, [uk, "vec", tk(0)], [tk(0)])
                ts("dve", t1, t1, GC1, 1.0, ALU.mult, ALU.add, [tk(8)], [tk(8)])
                tt("pool", t1, t1, gg, ALU.mult, [tk(8), tk(7)], [tk(8)])

            def lru_s2(c, tp):
                pr = (c * 4 + tp) % 2
                X = tmp[pr]
                tk = lambda i: f"tmp{pr}_{i}"
                tok = slice(tp * 512, (tp + 1) * 512)
                xc, gg, t1 = X[0], X[7], X[8]
                mm(PB(2), bdt[:, 0, c, :], xc, True, True, ["bdt", tk(0)], [pk(2)])
                mm(PB(3), bdt[:, 1, c, :], xc, True, True, ["bdt", tk(0)], [pk(3)])
                rr, ig, aa, ss, bb = X[1], X[2], X[3], X[4], X[5]
                act(rr, PB(2), AF.Sigmoid, [pk(2), "vec"], [tk(1)], bias=vec[:, V_BA + c:V_BA + c + 1], scale=1.0)
                act(ig, PB(3), AF.Sigmoid, [pk(3), "vec"], [tk(2)], bias=vec[:, V_BX + c:V_BX + c + 1], scale=1.0)
                act(t1, t1, AF.Sigmoid, [tk(8)], [tk(8)], scale=GC2)
                act(aa, rr, AF.Exp, [tk(1), "cl"], [tk(3)], scale=cl[:, c:c + 1])
                tt("pool", ss, aa, aa, ALU.mult, [tk(3)], [tk(4)])
                act(ss, ss, AF.Ln, [tk(4)], [tk(4)], bias=1.0, scale=-1.0)
                act(ss, ss, AF.Exp, [tk(4)], [tk(4)], scale=0.5)
                tt("pool", bb, ig, xc, ALU.mult, [tk(2), tk(0)], [tk(5)])
                tt("pool", bb, bb, ss, ALU.mult, [tk(5), tk(4)], [tk(5)])
                hh = hbuf[pr]
                hprev = hbuf[1 - pr]
                if tp == 0:
                    S.add("dve", lambda e, hh=hh, aa=aa, bb=bb: e.tensor_tensor_scan(out=hh, data0=aa, data1=bb, initial=0.0, op0=ALU.mult, op1=ALU.add),
                          [tk(3), tk(5)], [f"h{pr}"])
                else:
                    S.add("dve", lambda e, hh=hh, aa=aa, bb=bb, hprev=hprev: e.tensor_tensor_scan(out=hh, data0=aa, data1=bb, initial=hprev[:, 511:512], op0=ALU.mult, op1=ALU.add),
                          [tk(3), tk(5), f"h{1-pr}"], [f"h{pr}"])
                tt("pool", gg, gg, t1, ALU.mult, [tk(7), tk(8)], [tk(7)])
                yy = X[6]
                ri = (c * 4 + tp) % 4
                tt("dve", yy, hh, gg, ALU.mult, [f"h{pr}", tk(7)], [tk(6)])
                act(ysq_ring[ri], yy, AF.Square, [tk(6)], [f"ysq{ri}"])
                act(ylru[:, c, tok], yy, AF.Copy, [tk(6)], ["ylru"])

            def lru_s3(c, tp):
                ri = (c * 4 + tp) % 4
                mm(PB(4 + tp), ones_b, ysq_ring[ri], c == 0, c == 7, ["ones_b", f"ysq{ri}"], [pk(4 + tp)])

            lru_wload(0)
            pieces = [(c, tp) for c in range(8) for tp in range(4)]
            for i in range(len(pieces) + 3):
                if i < len(pieces):
                    lru_s1(*pieces[i])
                if 0 <= i - 1 < len(pieces):
                    lru_s2(*pieces[i - 1])
                if 0 <= i - 3 < len(pieces):
                    lru_s3(*pieces[i - 3])
            S.barrier()
            AR.reset(m_tmp)
            rl = AR.f32(T)
            ynb = [AR.bf(T) for _ in range(2)]
            for tp in range(4):
                act(rl[:, tp * 512:(tp + 1) * 512], PB(4 + tp), AF.Ln, [pk(4 + tp)], ["rl"], bias=EPS, scale=1.0 / 1024)
            act(rl, rl, AF.Exp, ["rl"], ["rl"], scale=-0.5)
            for c in range(8):
                yb = ynb[c % 2]
                stt(yb, ylru[:, c, :], vec[:, V_GLRU + c:V_GLRU + c + 1], rl, ALU.mult, ALU.mult, ["ylru", "vec", "rl"], [f"ynb{c%2}"])
                dma("sp", yT_s[sq, :, c, :], yb, [f"ynb{c%2}"], ["yT_s"])
            S.barrier()
            AR.reset(base_m)

            qT = AR.bf(4 * T).rearrange("p (r t) -> p r t", r=4)
            ksT = AR.bf(T)
            kwT = AR.bf(T)
            kcT = AR.bf(T)
            vcT = AR.bf(T)
            vsA = AR.bf(NTT * 65).rearrange("p (t d) -> p t d", t=NTT)
            vwA = AR.bf(NTT * 65).rearrange("p (t d) -> p t d", t=NTT)
            gsig = AR.f32(NTT * 12).rearrange("p (t c) -> p t c", t=NTT)
            kcmpT = AR.bf(128)
            vcA = AR.bf(98)
            hidT = AR.bf(2 * 128).rearrange("p (k n) -> p k n", k=2)
            w1c = AR.bf(32 * 256).rearrange("p (l h) -> p l h", l=32)
            w2c = AR.bf(2 * 64).rearrange("p (k d) -> p k d", k=2)
            peT = AR.bf(32)
            pbias = AR.f32(2)
            gtmp = [AR.f32(128) for _ in range(3)]
            Ec = AR.bf(16 * 4 * 128).rearrange("p (t r i) -> p t r i", t=16, r=4)
            ecs = AR.f32(4 * 4 * 128).rearrange("p (t r i) -> p t r i", t=4, r=4)
            wgrp = AR.bf(16 * 652).rearrange("p (k c) -> p k c", k=16)
            wq = wgrp[:, :, 0:256]
            wtok = wgrp[:, :, 512:652]
            NPT = 5
            Pt = [AR.bf(512) for _ in range(NPT)]
            etmp = [AR.f32(512) for _ in range(3)]
            otile = [AR.f32(256).rearrange("p (r d) -> p r d", r=4) for _ in range(2)]
            imp = AR.f32(32)
            impw = AR.f32(32)
            m8 = AR.f32(16)
            negm = [AR.bf(32) for _ in range(2)]
            sml = AR.f32(32)
            ot2b = {8: AR.f32(256), 12: AR.f32(256)}
            memset("dve", vsA[:, :, 64:65], 1.0, ["vsA"])
            memset("dve", vwA[:, :, 64:65], 1.0, ["vwA"])
            memset("dve", vcA[:, 64:65], 1.0, ["vcA"])
            dma("sp", vcA[0:127, 65:97], M_d, (), ["vcA"])
            dma("sp", ksT[64:96, :], ex_d, (), ["ksT"])

            def nsa_wload(g):
                dma("pool", wgrp, w_in_g[g].rearrange("(k p) c -> p k c", p=128), (), ["wgrp"])

            for g in range(4):
                if g == 0:
                    nsa_wload(0)
                for qq in range(4):
                    dma("sp", ecs, tbc[g, :, qq * 4:(qq + 1) * 4, :, :], (), ["ecs"])
                    for r in range(4):
                        act(Ec[:, qq * 4:(qq + 1) * 4, r, :], ecs[:, :, r, :], AF.Identity, ["ecs", "neg8c"], ["Ec"],
                            bias=neg8c[:, 4 * g + r:4 * g + r + 1], scale=8.0)
                fm_pairs = [(wgrp[:, :, 0:128], qT[0:64, 0, :], "qT", qT[0:64, 1, :], "qT"),
                            (wgrp[:, :, 128:256], qT[0:64, 2, :], "qT", qT[0:64, 3, :], "qT"),
                            (wgrp[:, :, 256:384], kcT[0:64, :], "kcT", vcT[0:64, :], "vcT"),
                            (wgrp[:, :, 384:512], ksT[0:64, :], "ksT", kwT[0:64, :], "kwT")]
                n_ = 0
                for (wsrc, dA, kA, dB, kB) in fm_pairs:
                    for tp in range(4):
                        b = n_ % 2
                        n_ += 1
                        tok = slice(tp * 512, (tp + 1) * 512)
                        for kc in range(16):
                            mm(PB(b), wsrc[:, kc, :], aT[:, kc, tok], kc == 0, kc == 15, ["wgrp", "aT"], [pk(b)])
                        act(dA[:, tok], PB(b)[0:64, :], AF.Copy, [pk(b)], [kA])
                        cp("dve", dB[:, tok], PB(b)[64:128, :], [pk(b)], [kB])
                for t in range(NTT):
                    b = 2 + t % 2
                    for kc in range(16):
                        mm(PB(b)[:, 0:140], aT[:, kc, t * 128:(t + 1) * 128], wtok[:, kc, :], kc == 0, kc == 15, ["aT", "wgrp"], [pk(b)])
                    cp("dve", vsA[:, t, 0:64], PB(b)[:, 0:64], [pk(b)], ["vsA"])
                    cp("dve", vwA[:, t, 0:64], PB(b)[:, 64:128], [pk(b)], ["vwA"])
                    act(gsig[:, t, :], PB(b)[:, 128:140], AF.Sigmoid, [pk(b)], ["gsig"])
                if g + 1 < 4:
                    nsa_wload(g + 1)
                for which, (cw1, cw2, pe_d, srcT) in enumerate(((cw1k, cw2k, pekT, kcT), (cw1v, cw2v, pevT, vcT))):
                    skey = "kcT" if which == 0 else "vcT"
                    dma("sp", w1c[0:64], ws_c1[which], ["ws_c1"], ["w1c"])
                    dma("pool", w2c, cw2.rearrange("(k p) d -> p k d", p=128), (), ["w2c"])
                    dma("pool", peT[0:64, :], pe_d, (), ["peT"])
                    for hk in range(2):
                        for l in range(32):
                            mm(PB(4)[:, 0:1], w1c[0:64, l, hk * 128:(hk + 1) * 128], peT[0:64, l:l + 1], l == 0, l == 31, ["w1c", "peT"], [pk(4)])
                        cp("dve", pbias[:, hk:hk + 1], PB(4)[:, 0:1], [pk(4)], ["pbias"])
                        for l in range(32):
                            mm(PB(5)[:, 0:127], w1c[0:64, l, hk * 128:(hk + 1) * 128], srcT[0:64, l:l + 16 * 126 + 1:16], l == 0, l == 31, ["w1c", skey], [pk(5)])
                        xg, t1, t2 = gtmp[0][:, 0:127], gtmp[1][:, 0:127], gtmp[2][:, 0:127]
                        act(xg, PB(5)[:, 0:127], AF.Identity, [pk(5), "pbias"], ["gt0"], bias=pbias[:, hk:hk + 1], scale=1.0)
                        act(t1, xg, AF.Square, ["gt0"], ["gt1"])
                        ts("dve", t1, t1, GC1, 1.0, ALU.mult, ALU.add, ["gt1"], ["gt1"])
                        tt("dve", t1, t1, xg, ALU.mult, ["gt1", "gt0"], ["gt1"])
                        act(t2, t1, AF.Sigmoid, ["gt1"], ["gt2"], scale=GC2)
                        tt("dve", hidT[:, hk, 0:127], xg, t2, ALU.mult, ["gt0", "gt2"], ["hidT"])
                    if which == 0:
                        for hk in range(2):
                            mm(PB(6)[0:64, 0:127], w2c[:, hk, :], hidT[:, hk, 0:127], hk == 0, hk == 1, ["w2c", "hidT"], [pk(6)])
                        cp("dve", kcmpT[0:64, 0:127], PB(6)[0:64, 0:127], [pk(6)], ["kcmpT"])
                    else:
                        for hk in range(2):
                            mm(PB(6)[0:127, 0:64], hidT[:, hk, 0:127], w2c[:, hk, :], hk == 0, hk == 1, ["w2c", "hidT"], [pk(6)])
                        cp("dve", vcA[0:127, 0:64], PB(6)[0:127, 0:64], [pk(6)], ["vcA"])

                sb_rr = [0]

                def sbank():
                    b = sb_rr[0] % 3
                    sb_rr[0] += 1
                    return b

                p_rr = [0]

                def pbuf():
                    i = p_rr[0] % NPT
                    p_rr[0] += 1
                    return i

                e_rr = [0]

                def ebuf():
                    i = e_rr[0] % 3
                    e_rr[0] += 1
                    return i

                def qsl(qt, rows):
                    return qT[0:rows, :, qt * 128:(qt + 1) * 128]

                def score_tile(lhsT, lkey, rows, qt, table, tkey, nk=128):
                    b = sbank()
                    qkeys = ["qT"] + ([f"qTm{qt}"] if rows > 64 else [])
                    ps3 = PB(b)[0:nk, :].rearrange("p (r q) -> p r q", r=4)
                    mm(ps3, lhsT, qsl(qt, rows), True, table is None, [lkey] + qkeys, [pk(b)])
                    if table is not None:
                        mm(ps3, ident[0:nk, 0:nk], table, False, True, ["ident", tkey], [pk(b)])
                    pi = pbuf()
                    act(Pt[pi][0:nk, :], PB(b)[0:nk, :], AF.Exp, [pk(b)], [f"P{pi}"], scale=0.125)
                    return pi

                jobs = []

                def jobC(qt):
                    op_ = qt % 2
                    nk = min(127, 8 * qt + 7)

                    def s1():
                        return score_tile(kcmpT[0:64, 0:nk], "kcmpT", 64, qt, Ec[0:nk, qt, :, :], "Ec", nk=nk)

                    def s2(pi):
                        acc = PB(3)[:, 0:388].rearrange("p (r c) -> p r c", r=4)
                        for r in range(4):
                            mm(acc[:, r, :], Pt[pi][0:nk, r * 128:(r + 1) * 128], vcA[0:nk, 0:97], True, True, [f"P{pi}", "vcA"], [pk(3)])
                        rden = sml[:, 0:4]
                        ts("dve", rden, acc[:, :, 64], 1e-30, None, ALU.max, None, [pk(3)], ["rdc"])
                        recip(rden, rden, ["rdc"], ["rdc"])
                        ts("dve", imp, acc[:, 0, 65:97], rden[:, 0:1], None, ALU.mult, None, [pk(3), "rdc"], ["imp"])
                        for r in range(1, 4):
                            stt(imp, acc[:, r, 65:97], rden[:, r:r + 1], imp, ALU.mult, ALU.add, [pk(3), "rdc", "imp"], ["imp"])
                        scc = sml[:, 4:8]
                        tt("dve", scc, rden, gsig[:, qt, :].rearrange("p (r c) -> p r c", r=4)[:, :, 0], ALU.mult, ["rdc", "gsig"], ["scc"])
                        tt("dve", otile[op_], acc[:, :, 0:64], scc.unsqueeze(2).to_broadcast([128, 4, 64]), ALU.mult, [pk(3), "scc"], [f"ot{op_}"])
                        tt("dve", imp, imp, cmfm[:, 0, qt, :], ALU.mult, ["imp", "cmfm"], ["imp"])
                        tt("dve", imp, imp, cmfm[:, 1, qt, :], ALU.add, ["imp", "cmfm"], ["imp"])
                        S.add("dve", lambda e: e.max(out=m8[:, 0:8], in_=imp), ["imp"], ["m8a"])
                        S.add("dve", lambda e: e.match_replace(out=impw, in_to_replace=m8[:, 0:8], in_values=imp, imm_value=-3.0e4), ["imp", "m8a"], ["impw"])
                        S.add("dve", lambda e: e.max(out=m8[:, 8:16], in_=impw), ["impw"], ["m8b"])
                        ts("dve", impw, imp, m8[:, 15:16], None, ALU.is_ge, None, ["imp", "m8b"], ["impw"])
                        ts("dve", negm[op_], impw, 1000.0, -1000.0, ALU.mult, ALU.add, ["impw"], [f"negm{op_}"])

                    jobs.append((s1, s2))

                def mask_T(qt):
                    op_ = qt % 2
                    ptn = PBb(3)[0:32, 896:1024]
                    tr(ptn, negm[op_], [f"negm{op_}"], [pk(3)])
                    cp("dve", qT[64:96, :, qt * 128:(qt + 1) * 128], ptn.unsqueeze(1).to_broadcast([32, 4, 128]), [pk(3)], [f"qTm{qt}"])

                def jobsKV(qt, kts, lhs_of, lkey, rows, vA, vkey, accb, tables, gcol, sc_off, pre=None, post=None):
                    op_ = qt % 2
                    acc = PB(accb)[:, 0:260].rearrange("p (r c) -> p r c", r=4)
                    for ii, kt in enumerate(kts):
                        first = ii == 0
                        last = ii == len(kts) - 1

                        def s1(kt=kt, first=first):
                            if first and pre is not None:
                                pre()
                            table, tkey = tables(kt)
                            return score_tile(lhs_of(kt), lkey, rows, qt, table, tkey)

                        def s2(pi, kt=kt, first=first, last=last):
                            if first:
                                mm(PB(accb)[:, 0:260], zer_b[:, 0:128], zer_b[:, 0:260], True, False, ["zer_b"], [pk(accb)], skip_group_check=True)
                            for r in range(4):
                                mm(acc[:, r, :], Pt[pi][:, r * 128:(r + 1) * 128], vA[:, kt, :], False, last, [f"P{pi}", vkey], [pk(accb)], skip_group_check=True)
                            if last:
                                rden = sml[:, sc_off:sc_off + 4]
                                kk = f"rd{sc_off}"
                                ts("dve", rden, acc[:, :, 64], 1e-30, None, ALU.max, None, [pk(accb)], [kk])
                                recip(rden, rden, [kk], [kk])
                                tt("dve", rden, rden, gsig[:, qt, :].rearrange("p (r c) -> p r c", r=4)[:, :, gcol], ALU.mult, [kk, "gsig"], [kk])
                                ot2 = ot2b[sc_off].rearrange("p (r d) -> p r d", r=4)
                                tt("dve", ot2, acc[:, :, 0:64], rden.unsqueeze(2).to_broadcast([128, 4, 64]), ALU.mult, [pk(accb), kk], ["ot2_" + str(sc_off)])
                                tt("dve", otile[op_], otile[op_], ot2, ALU.add, [f"ot{op_}", "ot2_" + str(sc_off)], [f"ot{op_}"])
                                if post is not None:
                                    post()

                        jobs.append((s1, s2))

                def en(d):
                    return enear[:, d, 4 * g:4 * g + 4, :]

                mwb = maskw4

                def tabW(qt):
                    def f(kt):
                        dl = qt - kt
                        if dl == 0:
                            return en(0), "enear"
                        if dl == 1:
                            return en(1), "enear"
                        if dl == 4:
                            return mwb, "maskw4"
                        return None, None
                    return f

                def tabS(qt):
                    def f(kt):
                        dl = qt - kt
                        if dl == 0:
                            return en(0), "enear"
                        if dl == 1:
                            return en(1), "enear"
                        return None, None
                    return f

                def out_dma(qt):
                    def f():
                        if qt + 1 < NTT:
                            mask_T(qt + 1)
                        dma("sp", onsa_s[sq, qt * 128:(qt + 1) * 128, g * 256:(g + 1) * 256], otile[qt % 2].rearrange("p r d -> p (r d)"), [f"ot{qt%2}"], ["onsa_s"])
                        pc = next(precast_it, None)
                        if pc is not None:
                            pc()
                    return f

                jobC(0)
                for qt in range(NTT):
                    if qt + 1 < NTT:
                        jobC(qt + 1)
                    ktsW = list(range(max(0, qt - 4), qt + 1))
                    jobsKV(qt, ktsW, lambda kt: kwT[0:64, kt * 128:(kt + 1) * 128], "kwT", 64, vwA, "vwA", 6 + qt % 2, tabW(qt), 2, 8)
                    ktsS = list(range(0, qt + 1))
                    jobsKV(qt, ktsS, lambda kt: ksT[0:96, kt * 128:(kt + 1) * 128], "ksT", 96, vsA, "vsA", 4 + qt % 2, tabS(qt), 1, 12,
                           pre=((lambda: mask_T(0)) if qt == 0 else None), post=out_dma(qt))
                LOOK = 2
                pis = {}
                for i in range(len(jobs) + LOOK):
                    if i < len(jobs):
                        pis[i] = jobs[i][0]()
                    j = i - LOOK
                    if j >= 0:
                        jobs[j][1](pis.pop(j))
            S.barrier()
            AR.reset(base_m)

            xt = [AR.f32(D) for _ in range(2)]
            junk = AR.bf(D)
            abf = [AR.bf(D) for _ in range(2)]
            ynT = [AR.bf(8 * 128).rearrange("p (k t) -> p k t", k=8) for _ in range(2)]
            for t in range(NTT):
                par = t % 2
                dma("sp", xt[par][:, 0:1024], onsa_s[sq, t * 128:(t + 1) * 128, :], ["onsa_s"], [f"xt{par}"])
                rms_to_T(xt[par][:, 0:1024], f"xt{par}", ynT[par], f"ynT{par}", 0, V_GNSA, par, 3 * par, nk=8)
                dma("sp", yT_s[sq, :, 8:16, t * 128:(t + 1) * 128], ynT[par], [f"ynT{par}"], ["yT_s"])
            S.barrier()
            AR.reset(0)
            if stop_after == "mixer":
                continue
            for pc in precast_it:
                pc()

            hT = AR.f32(4 * D).rearrange("p (t c) -> p t c", t=4)
            tmpT = AR.f32(4 * D).rearrange("p (t c) -> p t c", t=4)
            actT = AR.bf(16 * 512).rearrange("p (k t) -> p k t", k=16)
            hid = AR.bf(64 * 512).rearrange("p (k t) -> p k t", k=64)
            wbuf = [AR.bf(4096) for _ in range(3)]
            gr1 = AR.f32(D)
            abf = [AR.bf(D) for _ in range(2)]
            ptile = AR.f32(256)
            pb16 = AR.bf(256)
            pTt = AR.bf(2 * 512).rearrange("p (k t) -> p k t", k=2)
            rtmp = [AR.f32(512) for _ in range(2)]
            ssqp = AR.f32(64)
            wb_rr = [0]

            def next_wbuf():
                wi = wb_rr[0] % 3
                wb_rr[0] += 1
                return wi

            pre_w = [None]

            def prefetch_first(ws_src, wkey):
                wi = next_wbuf()
                wt_ = wbuf[wi][:, 0:4096].rearrange("p (k c) -> p k c", k=4)
                dma("sp", wt_, ws_src[0:512, 0:1024].rearrange("(k p) c -> p k c", p=128), [wkey], [f"wbuf{wi}"])
                pre_w[0] = (wi, wt_)

            def gemm_tok(ws_src, wkey, nkc, lhs_key, lhs_of, evac, hook=None, pre=None):
                for half in range(2):
                    for k4 in range(0, nkc, 4):
                        nkk = min(4, nkc - k4)
                        if pre is not None and half == 0 and k4 == 0:
                            wi, wt_ = pre
                        else:
                            wi = next_wbuf()
                            wt_ = wbuf[wi][:, 0:nkk * 1024].rearrange("p (k c) -> p k c", k=nkk)
                            dma("sp", wt_, ws_src[k4 * 128:(k4 + nkk) * 128, half * 1024:(half + 1) * 1024].rearrange("(k p) c -> p k c", p=128), [wkey], [f"wbuf{wi}"])
                        for kk in range(nkk):
                            kc = k4 + kk
                            for t4 in range(4):
                                for cb in range(2):
                                    b = t4 * 2 + cb
                                    mm(PB(b), lhs_of(kc, t4), wt_[:, kk, cb * 512:(cb + 1) * 512], kc == 0, kc == nkc - 1, [lhs_key, f"wbuf{wi}"], [pk(b)])
                        if hook is not None and half == 0 and k4 == 0:
                            hook()
                    for t4 in range(4):
                        for cb in range(2):
                            evac(half, t4, cb, PB(t4 * 2 + cb), t4 * 2 + cb)

            def evac_norm(half, t4, cb, ps, b):
                col = slice(half * 1024 + cb * 512, half * 1024 + (cb + 1) * 512)
                idx = t4 * 4 + half * 2 + cb
                if b % 2 == 0:
                    act(tmpT[:, t4, col], ps, AF.Copy, [pk(b)], [f"tmpT{t4}"])
                else:
                    cp("dve", tmpT[:, t4, col], ps, [pk(b)], [f"tmpT{t4}"])
                stt(rtmp[b % 2], tmpT[:, t4, col], 1.0, tmpT[:, t4, col], ALU.mult, ALU.mult, [f"tmpT{t4}"], [f"rtmp{b%2}", f"ssqp{idx}"],
                    accum_out=ssqp[:, idx:idx + 1])
                tt("pool", tmpT[:, t4, col], tmpT[:, t4, col], gr1[:, col], ALU.mult, [f"tmpT{t4}", "gr1"], [f"tmpT{t4}"])

            def post_norm_add(t4, st_off):
                ssq = stats[:, st_off:st_off + 1]
                kk = f"pn{st_off}"
                S.add("dve", lambda e: e.reduce_sum(out=ssq, in_=ssqp[:, t4 * 4:(t4 + 1) * 4], axis=mybir.AxisListType.X),
                      [f"ssqp{t4*4+i}" for i in range(4)], [kk])
                act(ssq, ssq, AF.Ln, [kk], [kk], bias=EPS, scale=1.0 / D)
                act(ssq, ssq, AF.Exp, [kk], [kk], scale=-0.5)
                stt(hT[:, t4, :], tmpT[:, t4, :], ssq, hT[:, t4, :], ALU.mult, ALU.add, [f"tmpT{t4}", kk, f"hT{t4}"], [f"hT{t4}"])

            for blk in range(4):
                tok0 = blk * 512
                if blk == 0:
                    dma("sp", hid[:, 0:16, :], yT_s[sq, :, :, tok0:tok0 + 512], ["yT_s"], ["hid"])

                def xload(tok0=tok0):
                    dma("sp", gr1, grow[:, 0, :], (), ["gr1"])
                    for t4 in range(4):
                        dma("sp", hT[:, t4, :], x[sq, tok0 + t4 * 128: tok0 + (t4 + 1) * 128, :], (), [f"hT{t4}"])

                gemm_tok(ws_out, "ws_out", 16, "hid", lambda kc, t4: hid[:, kc, t4 * 128:(t4 + 1) * 128], evac_norm, hook=xload, pre=pre_w[0])
                pre_w[0] = None
                for t4 in range(4):
                    post_norm_add(t4, 6 + t4)
                dma("sp", gr1, grow[:, 1, :], (), ["gr1"])
                for t4 in range(4):
                    rms_to_T(hT[:, t4, :], f"hT{t4}", actT, "actT", t4 * 128, V_GMLP, t4 % 2, 10 + 3 * (t4 % 2), pbs=(6, 7),
                             junk_ap=tmpT[:, t4, 0:1024].bitcast(BF16), junk_key=f"tmpT{t4}", scale_on_act=True)
                for h2 in range(32):
                    wi = next_wbuf()
                    wt_ = wbuf[wi].rearrange("p (k c) -> p k c", k=16)
                    dma("sp", wt_, ws_1[:, h2 * 256:(h2 + 1) * 256].rearrange("(k p) c -> p k c", p=128), ["ws_1"], [f"wbuf{wi}"])
                    for j in range(2):
                        hc = h2 * 2 + j
                        b = hc % 4
                        for kc in range(16):
                            mm(PB(b), wt_[:, kc, j * 128:(j + 1) * 128], actT[:, kc, :], kc == 0, kc == 15, [f"wbuf{wi}", "actT"], [pk(b)])
                        rt = rtmp[hc % 2]
                        act(rt, PB(b), AF.Relu, [pk(b)], [f"rtmp{hc%2}"])
                        tt("pool", hid[:, hc, :], rt, rt, ALU.mult, [f"rtmp{hc%2}"], ["hid"])
                gemm_tok(ws_2, "ws_2", 64, "hid", lambda kc, t4: hid[:, kc, t4 * 128:(t4 + 1) * 128], evac_norm)
                if blk + 1 < 4:
                    dma("sp", hid[:, 0:16, :], yT_s[sq, :, :, tok0 + 512:tok0 + 1024], ["yT_s"], ["hid"])
                for t4 in range(4):
                    post_norm_add(t4, 6 + t4)
                for t4 in range(4):
                    ab = abf[t4 % 2]
                    abk = f"abf{t4%2}"
                    act(ab, hT[:, t4, :], AF.Copy, [f"hT{t4}"], [abk])
                    for k4 in range(4):
                        b = 6 + k4 % 2
                        pt = PBb(b)[:, 0:512].rearrange("p (j q) -> p j q", j=4)
                        for j in range(4):
                            kc = k4 * 4 + j
                            tr(pt[:, j, :], ab[:, kc * 128:(kc + 1) * 128], [abk], [pk(b)])
                        cp("dve", actT[:, k4 * 4:(k4 + 1) * 4, t4 * 128:(t4 + 1) * 128], pt, [pk(b)], ["actT"])
                    dma("sp", ptile, pin[sq, tok0 + t4 * 128: tok0 + (t4 + 1) * 128, :], (), ["ptile"])
                    cp("pool", pb16, ptile, ["ptile"], ["pb16"])
                    pt = PBb(5)[:, 0:256].rearrange("p (j q) -> p j q", j=2)
                    for j in range(2):
                        tr(pt[:, j, :], pb16[:, j * 128:(j + 1) * 128], ["pb16"], [pk(5)])
                    cp("dve", pTt[:, :, t4 * 128:(t4 + 1) * 128], pt, [pk(5)], ["pTt"])

                def evac_sig(half, t4, cb, ps, b):
                    col = slice(half * 1024 + cb * 512, half * 1024 + (cb + 1) * 512)
                    act(tmpT[:, t4, col], ps, AF.Sigmoid, [pk(b)], [f"tmpT{t4}"])

                gemm_tok(ws_g, "ws_g", 16, "actT", lambda kc, t4: actT[:, kc, t4 * 128:(t4 + 1) * 128], evac_sig)
                for half in range(2):
                    wi = next_wbuf()
                    wpT = wbuf[wi][:, 0:2048].rearrange("p (k c) -> p k c", k=2)
                    dma("sp", wpT, ws_p[:, half * 1024:(half + 1) * 1024].rearrange("(k p) c -> p k c", p=128), ["ws_p"], [f"wbuf{wi}"])
                    for kc in range(2):
                        for t4 in range(4):
                            for cb in range(2):
                                b = t4 * 2 + cb
                                mm(PB(b), pTt[:, kc, t4 * 128:(t4 + 1) * 128], wpT[:, kc, cb * 512:(cb + 1) * 512], kc == 0, kc == 1, ["pTt", f"wbuf{wi}"], [pk(b)])
                    for t4 in range(4):
                        for cb in range(2):
                            b = t4 * 2 + cb
                            col = slice(half * 1024 + cb * 512, half * 1024 + (cb + 1) * 512)
                            tt("dve", tmpT[:, t4, col], tmpT[:, t4, col], PB(b), ALU.mult, [f"tmpT{t4}", pk(b)], [f"tmpT{t4}"])
                if blk + 1 < 4:
                    prefetch_first(ws_out, "ws_out")
                for t4 in range(4):
                    tt("pool", tmpT[:, t4, :], tmpT[:, t4, :], hT[:, t4, :], ALU.add, [f"tmpT{t4}", f"hT{t4}"], [f"tmpT{t4}"])
                    dma("sp", out[sq, tok0 + t4 * 128: tok0 + (t4 + 1) * 128, :], tmpT[:, t4, :], [f"tmpT{t4}"], ["out"])

        S.barrier(engines=("sp",))
        S.emit(st)
    return nc, S


def _bucket(dist):
    n = np.maximum(dist, 0)
    nf = np.maximum(n, 1).astype(np.float32)
    large = 16 + (np.log(nf / np.float32(16)) / np.float32(math.log(128 / 16)) * np.float32(16)).astype(np.int32)
    large = np.minimum(large, 31)
    return np.where(n < 16, n, large)


def _pm(v, n):
    return np.ascontiguousarray(np.asarray(v, np.float32).reshape(n, 128).T)


def _host_consts(inp):
    f = lambda k: np.asarray(inp[k], np.float32)
    rel = f("rel_bias")
    vecs = np.zeros((128, NVEC), np.float32)
    vecs[:, V_GPRE:V_GPRE + 16] = _pm(f("norm_mix_pre")[0], 16)
    vecs[:, V_GMLP:V_GMLP + 16] = _pm(f("norm_mlp_pre")[0], 16)
    vecs[:, V_GLRU:V_GLRU + 8] = _pm(f("gnorm_lru")[0], 8)
    vecs[:, V_GNSA:V_GNSA + 8] = _pm(f("gnorm_nsa")[0], 8)
    cw = f("conv_w")[0]
    vecs[:, V_CW:V_CW + 32] = cw.reshape(4, 8, 128).transpose(2, 1, 0).reshape(128, 32)
    vecs[:, V_CB:V_CB + 8] = _pm(f("conv_b")[0], 8)
    vecs[:, V_BA:V_BA + 8] = _pm(f("lru_ba")[0].reshape(-1), 8)
    vecs[:, V_BX:V_BX + 8] = _pm(f("lru_bx")[0].reshape(-1), 8)
    vecs[:, V_LAM:V_LAM + 8] = _pm(f("lru_lambda")[0], 8)
    vecs[:, V_C31:V_C31 + 16] = np.broadcast_to(rel[31][None, :], (128, 16))
    grow = np.ascontiguousarray(np.broadcast_to(np.stack([f("norm_mix_post")[0], f("norm_mlp_post")[0]])[None], (128, 2, D)))
    bdm = np.zeros((128, 2, 8, 128), np.float32)
    for a_, key in enumerate(("lru_wa", "lru_wx")):
        wmat = f(key)[0]
        for c in range(8):
            bdm[0:64, a_, c, 0:64] = wmat[2 * c]
            bdm[64:128, a_, c, 64:128] = wmat[2 * c + 1]
    k = np.arange(128)[:, None, None]
    dd = np.arange(2)[None, :, None]
    q = np.arange(128)[None, None, :]
    dist = q + 128 * dd - k
    tb = rel[_bucket(dist)]
    tb = np.where((dist >= 0)[..., None], tb, np.float32(-30000.0)).transpose(0, 1, 3, 2)
    tb_near = np.ascontiguousarray(tb.astype(np.float32))
    maskw = np.where(np.arange(128)[None, :] < np.arange(128)[:, None], 0.0, -240000.0).astype(ml_dtypes.bfloat16)
    n = np.arange(127)[:, None, None]
    qt = np.arange(16)[None, :, None]
    i = np.arange(128)[None, None, :]
    dc = 128 * qt + i - 16 * n - 31
    tbc = rel[_bucket(dc)]
    tbc = np.where((dc >= 0)[..., None], tbc, np.float32(-30000.0))
    tbc = tbc.reshape(127, 16, 128, 4, 4).transpose(3, 0, 1, 4, 2)
    tb_c = np.full((4, 128, 16, 4, 128), -30000.0, np.float32)
    tb_c[:, 0:127] = tbc
    ii = np.arange(128)[:, None, None]
    qq = np.arange(16)[None, :, None]
    jj = np.arange(32)[None, None, :]
    dblk = (128 * qq + ii) // 64 - jj
    forced = (jj == 0) | ((dblk >= 0) & (dblk < 2))
    causal = dblk >= 0
    cm = (causal & ~forced).astype(np.float32)
    fmv = np.where(forced, np.float32(1e4), np.where(causal, np.float32(0), np.float32(-1e4))).astype(np.float32)
    cmfm = np.ascontiguousarray(np.stack([cm, fmv], axis=1))
    ex = (np.arange(T)[None, :] // 64 == np.arange(32)[:, None]).astype(ml_dtypes.bfloat16)
    n_cmp, n_sel = 127, 32
    jjn = np.arange(n_sel)[:, None, None]
    ci = 4 * jjn + np.arange(4)[None, :, None] - np.arange(2)[None, None, :]
    jb = np.broadcast_to(jjn, ci.shape)
    ok = (ci >= 0) & (ci < n_cmp)
    Mm = np.zeros((n_cmp, n_sel), np.float32)
    np.add.at(Mm, (ci[ok], jb[ok]), 1.0)
    wi = f("w_in")[0]
    w_in_g = np.ascontiguousarray(np.stack([np.concatenate(
        [wi[:, IN_OFF["q"] + g * 256: IN_OFF["q"] + (g + 1) * 256]]
        + [wi[:, IN_OFF[nm] + g * 64: IN_OFF[nm] + (g + 1) * 64] for nm in ("kc", "vc", "ks", "kw", "vs", "vw")]
        + [wi[:, IN_OFF["gt"] + g * 12: IN_OFF["gt"] + (g + 1) * 12]], axis=1) for g in range(4)]))
    w_in_l = np.ascontiguousarray(np.stack([np.concatenate(
        [wi[:, c * 128:(c + 1) * 128], wi[:, 1024 + c * 128: 1024 + (c + 1) * 128]], axis=1) for c in range(8)]))
    return dict(
        vecs=vecs, grow=grow, bd=bdm, ident=np.eye(128, dtype=ml_dtypes.bfloat16), tb_near=tb_near, maskw=maskw,
        tb_c=tb_c, cmfm=cmfm, ex=ex, Mmat=Mm.astype(ml_dtypes.bfloat16),
        pekT=np.ascontiguousarray(f("cmp_pe_k")[0].T), pevT=np.ascontiguousarray(f("cmp_pe_v")[0].T),
        w_in_g=w_in_g, w_in_l=w_in_l, w_out=f("w_out")[0], mlp_w1=f("mlp_w1")[0], mlp_w2=f("mlp_w2")[0],
        ple_gate=f("ple_gate")[0], ple_proj=f("ple_proj")[0],
        cmp_w1_k=f("cmp_w1_k")[0], cmp_w1_v=f("cmp_w1_v")[0], cmp_w2_k=f("cmp_w2_k")[0], cmp_w2_v=f("cmp_w2_v")[0],
    )


_CACHE = {}


def kernel(**inputs):
    consts = _host_consts(inputs)
    x = np.asarray(inputs["x"], np.float32)
    p = np.asarray(inputs["p"], np.float32)
    n = 8
    if "nc" not in _CACHE:
        _CACHE["nc"] = build(nseq=2)[0]
    nc = _CACHE["nc"]
    in_maps = []
    for c in range(n):
        m = dict(consts)
        m["x"] = np.ascontiguousarray(x[2 * c:2 * c + 2])
        m["p"] = np.ascontiguousarray(p[0, 2 * c:2 * c + 2])
        in_maps.append(m)
    res = run_bass_kernel_spmd(nc, in_maps, core_ids=list(range(n)))
    return np.concatenate([np.asarray(r["out"], np.float32) for r in res.results], axis=0)
```
